# Optimizing a Trainium2 kernel written in Bass

```python
import math
import jax, jax.numpy as jnp
from jax import lax
import numpy as np

D_MODEL = 1024
BATCH = 1
SEQ = 16384
DEPTH = 2

DA_HEADS = 4
DA_WIDTH = D_MODEL // 2
DA_HEAD_DIM = DA_WIDTH // (2 * DA_HEADS)
HG_HEADS = 4
HG_WIDTH = D_MODEL // 4
HG_KEY_DIM = HG_WIDTH // HG_HEADS
HG_VAL_DIM = HG_WIDTH // HG_HEADS
HG_CHUNK = 64
POOL_WINDOWS = (2, 4, 8, 16)
POOL_WIDTH = D_MODEL // 4
POOL_GROUP = POOL_WIDTH // len(POOL_WINDOWS)
D_MIX = DA_WIDTH + HG_WIDTH + POOL_WIDTH
PROJ_SIZES = (DA_WIDTH, DA_WIDTH, DA_WIDTH, HG_HEADS * HG_KEY_DIM, HG_HEADS * HG_KEY_DIM, HG_WIDTH, HG_WIDTH, POOL_WIDTH)
D_IN = sum(PROJ_SIZES)
ATTN_BLOCK = 128
ROPE_THETA = 10000.0
MASK_VALUE = -1e30
TINY = 1e-30
FFN_DIM = 11 * D_MODEL // 4
N_EXPERTS = 8
TOP_K = 2
EXPERT_DIM = 7 * D_MODEL // 2
N_DENSE = (DEPTH + 1) // 2
N_MOE = DEPTH // 2
ALPHA = (2 * DEPTH) ** 0.25
BETA = (8 * DEPTH) ** -0.25
EPS = 1e-5

kernel_name = 'hybrid_diffattn_hgrn2_pool_moe_deepnorm_adaln'


def layer_norm(x, g, b):
    xf = x.astype(jnp.float32)
    mu = jnp.mean(xf, axis=-1, keepdims=True)
    var = jnp.mean(jnp.square(xf - mu), axis=-1, keepdims=True)
    return ((xf - mu) * lax.rsqrt(var + EPS)).astype(x.dtype) * g + b


def rms_norm(x, g):
    xf = x.astype(jnp.float32)
    return (xf * lax.rsqrt(jnp.mean(xf * xf, axis=-1, keepdims=True) + EPS)).astype(x.dtype) * g


def rotary(x, pos):
    d = x.shape[-1]
    inv_freq = 1.0 / (ROPE_THETA ** (jnp.arange(0, d, 2, dtype=jnp.float32) / d))
    ang = pos.astype(jnp.float32)[:, None] * inv_freq[None, :]
    cos = jnp.cos(ang)[None, :, None, :]
    sin = jnp.sin(ang)[None, :, None, :]
    xf = x.astype(jnp.float32)
    x1, x2 = xf[..., : d // 2], xf[..., d // 2:]
    return jnp.concatenate([x1 * cos - x2 * sin, x2 * cos + x1 * sin], axis=-1).astype(x.dtype)


def diff_attention(q, k, v, lam, lam_init, norm_g):
    B, S, NH2, d = q.shape
    H = NH2 // 2
    nblk = S // ATTN_BLOCK
    kT = k.transpose(0, 2, 1, 3)
    vT = v.transpose(0, 2, 1, 3)
    qb = (q * (d ** -0.5)).reshape(B, nblk, ATTN_BLOCK, NH2, d).transpose(1, 0, 3, 2, 4)
    key_pos = jnp.arange(S)

    def block(args):
        q_blk, start = args
        s = jnp.einsum('bnqd,bnkd->bnqk', q_blk, kT).astype(jnp.float32)
        q_pos = start + jnp.arange(ATTN_BLOCK)
        s = jnp.where(key_pos[None, :] <= q_pos[:, None], s, MASK_VALUE)
        p = jax.nn.softmax(s, axis=-1).reshape(B, H, 2, ATTN_BLOCK, S)
        a = p[:, :, 0] - lam * p[:, :, 1]
        return jnp.einsum('bhqk,bhkd->bhqd', a.astype(v.dtype), vT)

    starts = jnp.arange(nblk) * ATTN_BLOCK
    o = lax.map(block, (qb, starts))
    o = o.transpose(1, 0, 3, 2, 4).reshape(B, S, H, 2 * d)
    o = rms_norm(o, norm_g) * (1.0 - lam_init)
    return o.reshape(B, S, H * 2 * d)


def hgrn2(q, f_logit, i, g, lb, norm_g):
    B, S, H, dk = q.shape
    dv = i.shape[-1]
    C = HG_CHUNK
    nc = S // C
    zf = f_logit.astype(jnp.float32)
    lbh = lb.reshape(H, dk).astype(jnp.float32)
    f = lbh + (1.0 - lbh) * jax.nn.sigmoid(zf)
    log_f = jnp.log(jnp.maximum(f, TINY))
    key = (1.0 - lbh) * jax.nn.sigmoid(-zf)
    val = jax.nn.silu(i.astype(jnp.float32))
    qf = q.astype(jnp.float32)

    def to_chunks(t):
        return t.reshape(B, nc, C, H, t.shape[-1]).transpose(1, 0, 3, 2, 4)

    causal = jnp.tril(jnp.ones((C, C), dtype=bool))[None, None, :, :, None]

    def step(state, inp):
        qc, kc, vc, lfc = inp
        b = jnp.cumsum(lfc, axis=2)
        diff = b[:, :, :, None, :] - b[:, :, None, :, :]
        decay = jnp.where(causal, jnp.exp(jnp.where(causal, diff, 0.0)), 0.0)
        scores = jnp.sum(qc[:, :, :, None, :] * kc[:, :, None, :, :] * decay, axis=-1)
        o = jnp.einsum('bhts,bhsv->bhtv', scores, vc) + jnp.einsum('bhtk,bhkv->bhtv', qc * jnp.exp(b), state)
        b_last = b[:, :, -1:, :]
        new_state = jnp.exp(b_last[:, :, 0, :])[..., None] * state + jnp.einsum('bhsk,bhsv->bhkv', kc * jnp.exp(b_last - b), vc)
        return new_state, o

    state0 = jnp.zeros((B, H, dk, dv), jnp.float32)
    _, o = lax.scan(step, state0, (to_chunks(qf), to_chunks(key), to_chunks(val), to_chunks(log_f)))
    o = o.transpose(1, 0, 3, 2, 4).reshape(B, S, H, dv).astype(g.dtype)
    o = rms_norm(o, norm_g) * jax.nn.silu(g)
    return o.reshape(B, S, H * dv)


def multiscale_pool(u, w, scale):
    B, S, _ = u.shape
    uf = u.astype(jnp.float32)
    csum = jnp.pad(jnp.cumsum(uf, axis=1), ((0, 0), (1, 0), (0, 0)))
    t = jnp.arange(S)
    outs = []
    for gi, win in enumerate(POOL_WINDOWS):
        sl = slice(gi * POOL_GROUP, (gi + 1) * POOL_GROUP)
        cs = csum[:, :, sl]
        lo = jnp.maximum(t + 1 - win, 0)
        total = cs[:, 1:] - cs[:, lo]
        count = (t + 1 - lo).astype(jnp.float32)
        outs.append(total / count[None, :, None] - uf[:, :, sl])
    pooled = jnp.stack(outs, axis=2).astype(u.dtype)
    y = jnp.einsum('bsgc,gcd->bsgd', pooled, w)
    return y.reshape(B, S, POOL_WIDTH) * scale


def token_mixer(h, w_in, lam_qk, attn_g, lb, hg_g, pool_w, pool_scale, w_out, layer_idx, pos):
    B, S, _ = h.shape
    proj = h @ w_in
    splits = [int(v) for v in np.cumsum(PROJ_SIZES)[:-1]]
    q, k, v, hq, hf, hi, hgate, pu = jnp.split(proj, splits, axis=-1)
    q = rotary(q.reshape(B, S, 2 * DA_HEADS, DA_HEAD_DIM), pos)
    k = rotary(k.reshape(B, S, 2 * DA_HEADS, DA_HEAD_DIM), pos)
    v = v.reshape(B, S, DA_HEADS, 2 * DA_HEAD_DIM)
    lam_init = 0.8 - 0.6 * math.exp(-0.3 * layer_idx)
    lq = lam_qk.astype(jnp.float32)
    lam = jnp.exp(jnp.sum(lq[0] * lq[1])) - jnp.exp(jnp.sum(lq[2] * lq[3])) + lam_init
    a = diff_attention(q, k, v, lam, lam_init, attn_g)
    r = hgrn2(hq.reshape(B, S, HG_HEADS, HG_KEY_DIM), hf.reshape(B, S, HG_HEADS, HG_KEY_DIM),
              hi.reshape(B, S, HG_HEADS, HG_VAL_DIM), hgate.reshape(B, S, HG_HEADS, HG_VAL_DIM), lb, hg_g)
    p = multiscale_pool(pu, pool_w, pool_scale)
    return jnp.concatenate([a, r, p], axis=-1) @ w_out


def swiglu(h, w1, w3, w2):
    return (jax.nn.silu(h @ w1) * (h @ w3)) @ w2


def moe_ffn(h, router_w, w1, w3, w2):
    logits = (h @ router_w).astype(jnp.float32)
    top_v, top_i = lax.top_k(logits, TOP_K)
    gates = jax.nn.softmax(top_v, axis=-1)
    combine = jnp.sum(jax.nn.one_hot(top_i, N_EXPERTS, dtype=jnp.float32) * gates[..., None], axis=-2)
    y = jnp.zeros_like(h)
    for e in range(N_EXPERTS):
        y = y + combine[..., e:e + 1].astype(h.dtype) * swiglu(h, w1[e], w3[e], w2[e])
    return y


def setup_inputs(seed: int = 0) -> dict:
    key = jax.random.key(seed)
    ks = jax.random.split(key, 24)
    D = D_MODEL

    def nrm(k, shape, std):
        return jax.random.normal(k, shape, jnp.float32) * std

    return {
        'x': nrm(ks[0], (BATCH, SEQ, D), 1.0),
        'c': nrm(ks[1], (BATCH, D), 1.0),
        'w_ada': nrm(ks[2], (DEPTH, D, 6 * D), 0.1 * D ** -0.5),
        'b_ada': nrm(ks[3], (DEPTH, 6 * D), 0.02),
        'w_in': nrm(ks[4], (DEPTH, D, D_IN), D ** -0.5),
        'lam_qk': nrm(ks[5], (DEPTH, 4, DA_HEAD_DIM), 0.1),
        'attn_norm_g': 1.0 + nrm(ks[6], (DEPTH, 2 * DA_HEAD_DIM), 0.02),
        'hg_lb_logits': nrm(ks[7], (DEPTH, HG_HEADS * HG_KEY_DIM), 0.5),
        'hg_norm_g': 1.0 + nrm(ks[8], (DEPTH, HG_VAL_DIM), 0.02),
        'pool_w': nrm(ks[9], (DEPTH, len(POOL_WINDOWS), POOL_GROUP, POOL_GROUP), POOL_GROUP ** -0.5),
        'pool_scale': 1.0 + nrm(ks[10], (DEPTH, POOL_WIDTH), 0.02),
        'w_out': nrm(ks[11], (DEPTH, D_MIX, D), BETA * D_MIX ** -0.5),
        'ln1_g': 1.0 + nrm(ks[12], (DEPTH, D), 0.02),
        'ln1_b': nrm(ks[13], (DEPTH, D), 0.02),
        'ln2_g': 1.0 + nrm(ks[14], (DEPTH, D), 0.02),
        'ln2_b': nrm(ks[15], (DEPTH, D), 0.02),
        'ffn_w1': nrm(ks[16], (N_DENSE, D, FFN_DIM), D ** -0.5),
        'ffn_w3': nrm(ks[17], (N_DENSE, D, FFN_DIM), D ** -0.5),
        'ffn_w2': nrm(ks[18], (N_DENSE, FFN_DIM, D), BETA * FFN_DIM ** -0.5),
        'router_w': nrm(ks[19], (N_MOE, D, N_EXPERTS), D ** -0.5),
        'exp_w1': nrm(ks[20], (N_MOE, N_EXPERTS, D, EXPERT_DIM), D ** -0.5),
        'exp_w3': nrm(ks[21], (N_MOE, N_EXPERTS, D, EXPERT_DIM), D ** -0.5),
        'exp_w2': nrm(ks[22], (N_MOE, N_EXPERTS, EXPERT_DIM, D), BETA * EXPERT_DIM ** -0.5),
    }


def reference(x, c, w_ada, b_ada, w_in, lam_qk, attn_norm_g, hg_lb_logits, hg_norm_g, pool_w, pool_scale,
              w_out, ln1_g, ln1_b, ln2_g, ln2_b, ffn_w1, ffn_w3, ffn_w2, router_w, exp_w1, exp_w3, exp_w2):
    S = x.shape[1]
    pos = jnp.arange(S)
    P = jax.nn.softmax(hg_lb_logits.astype(jnp.float32), axis=0)
    lbs = jnp.cumsum(P, axis=0) - P[0:1]
    cond = jax.nn.silu(c)
    for l in range(DEPTH):
        mod = (cond @ w_ada[l] + b_ada[l])[:, None, :]
        sh1, sc1, g1, sh2, sc2, g2 = jnp.split(mod, 6, axis=-1)
        h = x * (1.0 + sc1) + sh1
        m = token_mixer(h, w_in[l], lam_qk[l], attn_norm_g[l], lbs[l], hg_norm_g[l], pool_w[l], pool_scale[l],
                        w_out[l], l, pos)
        x = layer_norm(ALPHA * x + (1.0 + g1) * m, ln1_g[l], ln1_b[l])
        h = x * (1.0 + sc2) + sh2
        if l % 2 == 0:
            f = swiglu(h, ffn_w1[l // 2], ffn_w3[l // 2], ffn_w2[l // 2])
        else:
            f = moe_ffn(h, router_w[l // 2], exp_w1[l // 2], exp_w3[l // 2], exp_w2[l // 2])
        x = layer_norm(ALPHA * x + (1.0 + g2) * f, ln2_g[l], ln2_b[l])
    return x
```

```python
import numpy as np
from contextlib import ExitStack
import concourse.bass as bass
import concourse.mybir as mybir
from concourse.bass_utils import run_bass_kernel_spmd

F32 = mybir.dt.float32
BF16 = mybir.dt.bfloat16
AF = mybir.ActivationFunctionType
ALU = mybir.AluOpType
AX = mybir.AxisListType


class _Op:
    __slots__ = ("eng", "fn", "deps", "dma", "ndma", "signal", "sem", "val", "idx")


class Prog:
    ENGS = ("pe", "act", "dve", "pool", "sp")

    def __init__(self):
        self.nc = bass.Bass("TRN2", target_bir_lowering=False)
        self.es = ExitStack()
        self.ops = []
        self.last_w = {}
        self.readers = {}
        self.bar = None
        self.scopes = []
        self._bar_t = self.es.enter_context(self.nc.sbuf_tensor("sb__bar", [1, 8], F32))

    def sb(self, name, shape, dt):
        es = self.scopes[-1] if self.scopes else self.es
        return es.enter_context(self.nc.sbuf_tensor("sb_" + name, list(shape), dt))

    def open_scope(self):
        self.scopes.append(ExitStack())

    def close_scope(self):
        self.scopes.pop().close()
        self.barrier()

    def barrier(self):
        keys = list(set(list(self.last_w.keys()) + list(self.readers.keys())))
        t = self._bar_t
        o = self.op("dve", lambda e: e.memset(t[:], 0.0), reads=keys, writes=["__bar"])
        self.bar = o.idx

    def ps(self, name, shape, dt):
        return self.es.enter_context(self.nc.psum_tensor("ps_" + name, list(shape), dt))

    def dram(self, name, shape, dt, kind="Internal"):
        return self.nc.dram_tensor(name, list(shape), dt, kind=kind).ap()

    def op(self, eng, fn, reads=(), writes=(), dma=None, ndma=1):
        o = _Op()
        o.eng = eng
        o.fn = fn
        o.dma = dma
        o.ndma = ndma if dma is not None else 0
        o.signal = False
        o.sem = None
        o.val = 0
        o.idx = len(self.ops)
        deps = set()
        for k in reads:
            w = self.last_w.get(k)
            if w is not None:
                deps.add(w)
        for k in writes:
            w = self.last_w.get(k)
            if w is not None:
                deps.add(w)
            for r in self.readers.get(k, ()):
                deps.add(r)
        if self.bar is not None:
            deps.add(self.bar)
        deps.discard(o.idx)
        o.deps = deps
        for k in reads:
            self.readers.setdefault(k, []).append(o.idx)
        for k in writes:
            self.last_w[k] = o.idx
            self.readers[k] = []
        self.ops.append(o)
        return o

    def pe(self, fn, reads=(), writes=()):
        return self.op("pe", fn, reads, writes)

    def act(self, fn, reads=(), writes=()):
        return self.op("act", fn, reads, writes)

    def dve(self, fn, reads=(), writes=()):
        return self.op("dve", fn, reads, writes)

    def pool(self, fn, reads=(), writes=()):
        return self.op("pool", fn, reads, writes)

    def dma(self, group, fn, reads=(), writes=(), eng="sp", ndma=1):
        return self.op(eng, fn, reads, writes, dma=group, ndma=ndma)

    def emit(self):
        nc = self.nc
        ops = self.ops

        def needs(d, o):
            if d.dma is not None:
                return True
            if d.eng == "pe" and o.eng == "pe" and o.dma is None:
                return False
            return True

        for o in ops:
            for di in o.deps:
                d = ops[di]
                if needs(d, o):
                    d.signal = True
        cnt = {}
        for o in ops:
            if o.dma is not None:
                key = "d:" + o.dma
                cnt[key] = cnt.get(key, 0) + 16 * o.ndma
                o.sem = key
                o.val = cnt[key]
            elif o.signal:
                key = "e:" + o.eng
                cnt[key] = cnt.get(key, 0) + 1
                o.sem = key
                o.val = cnt[key]
        sems = {}
        for key in cnt:
            sems[key] = self.es.enter_context(nc.semaphore("s_" + key.replace(":", "_")))
        self.nsem = len(sems)
        block = self.es.enter_context(nc.Block())
        by_eng = {e: [o for o in ops if o.eng == e] for e in self.ENGS}

        def run_stream(engname, e):
            waited = {}
            for o in by_eng[engname]:
                need = {}
                for di in o.deps:
                    d = ops[di]
                    if not needs(d, o):
                        continue
                    if need.get(d.sem, 0) < d.val:
                        need[d.sem] = d.val
                for s, v in need.items():
                    if waited.get(s, 0) < v:
                        e.wait_ge(sems[s], v)
                        waited[s] = v
                if o.fn is None:
                    continue
                r = o.fn(e)
                if o.dma is not None:
                    ins = r if isinstance(r, (list, tuple)) else [r]
                    assert len(ins) == o.ndma, (len(ins), o.ndma)
                    for i in ins:
                        i.then_inc(sems[o.sem], 16)
                elif o.signal:
                    i = r[-1] if isinstance(r, (list, tuple)) else r
                    i.then_inc(sems[o.sem], 1)

        if by_eng["pe"]:
            @block.tensor
            def _(e):
                run_stream("pe", e)
        if by_eng["act"]:
            @block.scalar
            def _(e):
                run_stream("act", e)
        if by_eng["dve"]:
            @block.vector
            def _(e):
                run_stream("dve", e)
        if by_eng["pool"]:
            @block.gpsimd
            def _(e):
                run_stream("pool", e)
        if by_eng["sp"]:
            @block.sync
            def _(e):
                run_stream("sp", e)
        self.es.close()
        return nc

import math

D=1024
def rope_tables(pos):
    inv = (1.0 / (np.float32(10000.0) ** (np.arange(0, 64, 2, dtype=np.float32) / np.float32(64)))).astype(np.float32)
    ang = pos.astype(np.float32)[:, None] * inv[None, :]
    c = np.cos(ang.astype(np.float64)).astype(np.float32)
    s = np.sin(ang.astype(np.float64)).astype(np.float32)
    cos64 = np.concatenate([c, c], 1)
    sin64 = np.concatenate([-s, s], 1)
    cosT = np.ascontiguousarray(np.concatenate([cos64, cos64], 1).T)
    sinT = np.ascontiguousarray(np.concatenate([sin64, sin64], 1).T)
    return cosT, sinT

def win_perm(w_in_l):
    q, k, v, hq, hf, hi, hg, u = np.split(w_in_l, [512, 1024, 1536, 1792, 2048, 2304, 2560], axis=1)
    def sw(w):
        w4 = w.reshape(1024, 8, 2, 32)
        return w4[:, :, ::-1, :].reshape(1024, 512)
    return np.ascontiguousarray(np.concatenate([q, k, v, hq, hf, u, hi, hg, sw(q), sw(k)], 1))

def hgrn_consts():
    s = np.arange(64)[:, None]; t = np.arange(64)[None, :]
    tric = (s <= t).astype(np.float32) - (s <= 31).astype(np.float32)
    ext = np.stack([(np.arange(64) <= 31), np.ones(64), (np.arange(64) > 31), np.zeros(64)], 1).astype(np.float32)
    return np.ascontiguousarray(np.concatenate([tric, ext], 1))

POOL_WINDOWS = (2, 4, 8, 16)
def pool_mats(first):
    cur = np.zeros((128, 4, 128), np.float32); prev = np.zeros((128, 4, 128), np.float32)
    for g, win in enumerate(POOL_WINDOWS):
        for t in range(128):
            cnt = min(win, t + 1) if first else win
            for d in range(cnt):
                s = t - d
                if s >= 0:
                    cur[s, g, t] += 1.0 / cnt
                else:
                    prev[128 + s, g, t] += 1.0 / cnt
            cur[t, g, t] -= 1.0
    return cur, prev


D = 1024
NW = 3840
C_Q, C_K, C_V, C_HQ, C_HF, C_U, C_HI, C_HG, C_QS, C_KS = 0, 512, 1024, 1536, 1792, 2048, 2304, 2560, 2816, 3328


class RR:
    def __init__(self, items):
        self.items = items
        self.i = 0

    def next(self):
        it = self.items[self.i % len(self.items)]
        self.i += 1
        return it


def make_ident(P, dt=BF16):
    identf = P.sb("identf", [128, 128], F32)
    ident = P.sb("ident", [128, 128], dt)

    def mk(e):
        a = e.memset(identf[:], 1.0)
        b = e.affine_select(out=identf[:], in_=identf[:], pattern=[[-1, 128]], compare_op=ALU.is_equal,
                            fill=0.0, base=0, channel_multiplier=1)
        return [a, b]
    P.pool(mk, writes=["identf"])
    P.dve(lambda e: e.tensor_copy(out=ident[:], in_=identf[:]), reads=["identf"], writes=["ident"])
    return ident, identf


def build_p1(T, layer):
    P = Prog()
    G = T // 512
    x_d = P.dram("x", [T, D], F32, "ExternalInput")
    ct_d = P.dram("ct", [128, 8], F32, "ExternalInput")
    wada_d = P.dram("wada", [D, 2048], F32, "ExternalInput")
    bada_d = P.dram("bada", [2048], F32, "ExternalInput")
    win_d = P.dram("win", [D, NW], F32, "ExternalInput")
    cos_d = P.dram("cosT", [128, T], F32, "ExternalInput")
    sin_d = P.dram("sinT", [128, T], F32, "ExternalInput")
    lbl_d = P.dram("lbl", [2, 256], F32, "ExternalInput")
    QT_d = P.dram("QT", [512, T], BF16, "ExternalOutput")
    KT_d = P.dram("KT", [512, T], BF16, "ExternalOutput")
    V_d = P.dram("V", [T, 512], BF16, "ExternalOutput")
    HQT_d = P.dram("HQT", [256, T], F32, "ExternalOutput")
    SGT_d = P.dram("SGT", [256, T], F32, "ExternalOutput")
    LOGF_d = P.dram("LOGF", [T, 256], F32, "ExternalOutput")
    VAL_d = P.dram("VAL", [T, 256], BF16, "ExternalOutput")
    SG_d = P.dram("SG", [T, 256], F32, "ExternalOutput")
    U_d = P.dram("U", [T, 256], F32, "ExternalOutput")
    outs = []

    ident, _ = make_ident(P)
    ones = P.sb("ones", [128, 1], F32)
    P.pool(lambda e: e.memset(ones[:], 1.0), writes=["ones"])

    wbf = P.sb("wbf", [128, 8, NW], BF16)
    for j in range(8):
        P.dma("wbf%d" % j, lambda e, j=j: e.dma_start(out=wbf[:, j, :], in_=win_d[j * 128:(j + 1) * 128, :]),
              writes=["wbf%d" % j], eng="pool")
    wkeys = ["wbf%d" % j for j in range(8)]

    ct = P.sb("ct", [128, 8], F32)
    cond = P.sb("cond", [128, 8], F32)
    condb = P.sb("condb", [128, 8, 128], F32)
    P.dma("ct", lambda e: e.dma_start(out=ct[:], in_=ct_d), writes=["ct"])
    P.act(lambda e: e.activation(out=cond[:], in_=ct[:], func=AF.Silu), reads=["ct"], writes=["cond"])
    P.dve(lambda e: e.tensor_copy(out=condb[:], in_=cond[:].unsqueeze(2).to_broadcast([128, 8, 128])),
          reads=["cond"], writes=["condb"])
    mod = P.sb("mod", [128, 2048], F32)
    badab = P.sb("badab", [128, 2048], F32)
    P.dma("badab", lambda e: e.dma_start(out=badab[:], in_=bada_d.partition_broadcast(128)), writes=["badab"])
    wst = [("wst%d" % i, P.sb("wst%d" % i, [128, 512], F32)) for i in range(3)]
    wrr = RR(wst)
    pbs = [("pb%d" % i, P.ps("pb%d" % i, [128, 512], F32)) for i in range(7)]
    prr = RR(pbs)
    psT = P.ps("psT", [128, 1024], BF16)
    for n in range(4):
        pk, pb = prr.next()
        for k in range(8):
            wk, wt = wrr.next()
            P.dma(wk, lambda e, wt=wt, k=k, n=n: e.dma_start(out=wt[:], in_=wada_d[k * 128:(k + 1) * 128, n * 512:(n + 1) * 512]),
                  writes=[wk])
            P.pe(lambda e, pb=pb, wt=wt, k=k: e.matmul(pb[:], condb[:, k, :], wt[:], start=(k == 0), stop=(k == 7)),
                 reads=[wk, "condb"], writes=[pk])
        P.dve(lambda e, pb=pb, n=n: e.tensor_tensor(out=mod[:, n * 512:(n + 1) * 512], in0=pb[:], in1=badab[:, n * 512:(n + 1) * 512], op=ALU.add),
              reads=[pk, "badab"], writes=["mod"])
    P.dve(lambda e: e.tensor_scalar_add(out=mod[:, 1024:2048], in0=mod[:, 1024:2048], scalar1=1.0), reads=["mod"], writes=["mod"])

    cosT = P.sb("cosT_s", [128, T], F32)
    sinT = P.sb("sinT_s", [128, T], F32)
    P.dma("cosT", lambda e: e.dma_start(out=cosT[:], in_=cos_d), writes=["cosT"])
    P.dma("sinT", lambda e: e.dma_start(out=sinT[:], in_=sin_d), writes=["sinT"])

    if layer == 1:
        lbb = P.sb("lbb", [128, 2, 256], F32)
        lb = P.sb("lb", [128, 256], F32)
        P.dma("lbb", lambda e: e.dma_start(out=lbb[:], in_=lbl_d.rearrange("a b -> (a b)").partition_broadcast(128)), writes=["lbb"])
        P.dve(lambda e: e.tensor_tensor(out=lb[:], in0=lbb[:, 1, :], in1=lbb[:, 0, :], op=ALU.subtract), reads=["lbb"], writes=["lb"])
        P.act(lambda e: e.activation(out=lb[:], in_=lb[:], func=AF.Sigmoid), reads=["lb"], writes=["lb"])

    xrr = RR([("xt%d" % i, P.sb("xt%d" % i, [128, D], F32)) for i in range(3)])
    hrr = RR([("hb%d" % i, P.sb("hb%d" % i, [128, D], BF16)) for i in range(2)])
    htrr = RR([("hT%d" % i, P.sb("hT%d" % i, [128, 8, 512], BF16)) for i in range(2)])
    tmprr = RR([("tm%d" % i, P.sb("tm%d" % i, [128, D], F32)) for i in range(2)])
    vrr = RR([("vt%d" % i, P.sb("vt%d" % i, [128, 512], BF16)) for i in range(2)])
    err_ = RR([("et%d" % i, P.sb("et%d" % i, [128, 256], F32)) for i in range(2)])
    l1rr = RR([("l1%d" % i, P.sb("l1%d" % i, [128, 256], F32)) for i in range(2)])
    l2rr = RR([("l2%d" % i, P.sb("l2%d" % i, [128, 256], F32)) for i in range(2)])
    lfrr = RR([("lf%d" % i, P.sb("lf%d" % i, [128, 256], F32)) for i in range(2)])
    valrr = RR([("va%d" % i, P.sb("va%d" % i, [128, 256], BF16)) for i in range(2)])
    sgrr = RR([("sg%d" % i, P.sb("sg%d" % i, [128, 256], F32)) for i in range(2)])
    urr = RR([("ut%d" % i, P.sb("ut%d" % i, [128, 256], F32)) for i in range(2)])
    r1rr = RR([("r1%d" % i, P.sb("r1%d" % i, [128, 512], F32)) for i in range(2)])
    r2rr = RR([("r2%d" % i, P.sb("r2%d" % i, [128, 512], F32)) for i in range(2)])
    qrrr = RR([("qr%d" % i, P.sb("qr%d" % i, [128, 512], BF16)) for i in range(3)])
    fmrr = RR([("fm%d" % i, P.sb("fm%d" % i, [128, 512], F32)) for i in range(3)])

    for g in range(G):
        hk, hT = htrr.next()
        for tt in range(4):
            r0 = g * 512 + tt * 128
            xk, xt = xrr.next()
            P.dma(xk, lambda e, xt=xt, r0=r0: e.dma_start(out=xt[:], in_=x_d[r0:r0 + 128, :]), writes=[xk])
            tk, tm = tmprr.next()
            bk, hb = hrr.next()
            P.dve(lambda e, tm=tm, xt=xt: e.tensor_tensor(out=tm[:], in0=xt[:], in1=mod[:, 1024:2048], op=ALU.mult),
                  reads=[xk, "mod"], writes=[tk])
            P.pool(lambda e, tm=tm, hb=hb: e.tensor_tensor(out=hb[:], in0=tm[:], in1=mod[:, 0:1024], op=ALU.add),
                   reads=[tk, "mod"], writes=[bk])
            P.pe(lambda e, hb=hb: [e.transpose(psT[:, k * 128:(k + 1) * 128], hb[:, k * 128:(k + 1) * 128], ident[:]) for k in range(8)],
                 reads=[bk, "ident"], writes=["psT"])
            P.act(lambda e, hT=hT, tt=tt: e.copy(out=hT[:, :, tt * 128:(tt + 1) * 128], in_=psT[:].rearrange("p (k t) -> p k t", k=8)),
                  reads=["psT"], writes=[hk])
            pkv, pbv = prr.next()
            P.pe(lambda e, pbv=pbv, hT=hT, tt=tt: [e.matmul(pbv[:], hT[:, k, tt * 128:(tt + 1) * 128], wbf[:, k, C_V:C_V + 512], start=(k == 0), stop=(k == 7)) for k in range(8)],
                 reads=[hk] + wkeys, writes=[pkv])
            vk, vt = vrr.next()
            P.act(lambda e, vt=vt, pbv=pbv: e.copy(out=vt[:], in_=pbv[:]), reads=[pkv], writes=[vk])
            P.dma(vk, lambda e, vt=vt, r0=r0: e.dma_start(out=V_d[r0:r0 + 128, :], in_=vt[:]), reads=[vk], writes=["oV%d" % r0])
            outs.append("oV%d" % r0)
            pkb, pbb = prr.next()
            P.pe(lambda e, pbb=pbb, hT=hT, tt=tt: [e.matmul(pbb[:], hT[:, k, tt * 128:(tt + 1) * 128], wbf[:, k, C_HF:C_HF + 512], start=(k == 0), stop=(k == 7)) for k in range(8)],
                 reads=[hk] + wkeys, writes=[pkb])
            ek, et = err_.next()
            P.act(lambda e, et=et, pbb=pbb: e.activation(out=et[:], in_=pbb[:, 0:256], func=AF.Exp, scale=-1.0), reads=[pkb], writes=[ek])
            uk, ut = urr.next()
            P.dve(lambda e, ut=ut, pbb=pbb: e.tensor_copy(out=ut[:], in_=pbb[:, 256:512]), reads=[pkb], writes=[uk])
            P.dma(uk, lambda e, ut=ut, r0=r0: e.dma_start(out=U_d[r0:r0 + 128, :], in_=ut[:]), reads=[uk], writes=["oU%d" % r0])
            outs.append("oU%d" % r0)
            l1k, l1 = l1rr.next()
            P.act(lambda e, l1=l1, et=et: e.activation(out=l1[:], in_=et[:], func=AF.Ln, bias=ones[:], scale=1.0), reads=[ek, "ones"], writes=[l1k])
            lfk, lf = lfrr.next()
            if layer == 0:
                P.pool(lambda e, lf=lf, l1=l1: e.tensor_scalar(out=lf[:], in0=l1[:], scalar1=-1.0, scalar2=None, op0=ALU.mult), reads=[l1k], writes=[lfk])
            else:
                l2k, l2 = l2rr.next()
                P.pool(lambda e, l2=l2, et=et: e.tensor_tensor(out=l2[:], in0=et[:], in1=lb[:], op=ALU.mult), reads=[ek, "lb"], writes=[l2k])
                P.act(lambda e, l2=l2: e.activation(out=l2[:], in_=l2[:], func=AF.Ln, bias=ones[:], scale=1.0), reads=[l2k, "ones"], writes=[l2k])
                P.pool(lambda e, lf=lf, l1=l1, l2=l2: e.tensor_tensor(out=lf[:], in0=l2[:], in1=l1[:], op=ALU.subtract), reads=[l1k, l2k], writes=[lfk])
            P.dma(lfk, lambda e, lf=lf, r0=r0: e.dma_start(out=LOGF_d[r0:r0 + 128, :], in_=lf[:]), reads=[lfk], writes=["oLF%d" % r0])
            outs.append("oLF%d" % r0)
            pkc, pbc = prr.next()
            P.pe(lambda e, pbc=pbc, hT=hT, tt=tt: [e.matmul(pbc[:], hT[:, k, tt * 128:(tt + 1) * 128], wbf[:, k, C_HI:C_HI + 512], start=(k == 0), stop=(k == 7)) for k in range(8)],
                 reads=[hk] + wkeys, writes=[pkc])
            vak, va = valrr.next()
            sgk, sg = sgrr.next()
            P.act(lambda e, va=va, pbc=pbc: e.activation(out=va[:], in_=pbc[:, 0:256], func=AF.Silu), reads=[pkc], writes=[vak])
            P.act(lambda e, sg=sg, pbc=pbc: e.activation(out=sg[:], in_=pbc[:, 256:512], func=AF.Silu), reads=[pkc], writes=[sgk])
            P.dma(vak, lambda e, va=va, r0=r0: e.dma_start(out=VAL_d[r0:r0 + 128, :], in_=va[:]), reads=[vak], writes=["oVA%d" % r0])
            P.dma(sgk, lambda e, sg=sg, r0=r0: e.dma_start(out=SG_d[r0:r0 + 128, :], in_=sg[:]), reads=[sgk], writes=["oSG%d" % r0])
            outs += ["oVA%d" % r0, "oSG%d" % r0]
        c0 = g * 512

        def fm_mm(pb, col, hT=hT):
            return lambda e: [e.matmul(pb[:], wbf[:, k, col:col + 128], hT[:, k, :], start=(k == 0), stop=(k == 7)) for k in range(8)]
        for which, cbase, csw, out_d in (("q", C_Q, C_QS, QT_d), ("k", C_K, C_KS, KT_d)):
            for mp in range(4):
                pk1, pb1 = prr.next()
                pk2, pb2 = prr.next()
                P.pe(fm_mm(pb1, cbase + mp * 128), reads=[hk] + wkeys, writes=[pk1])
                P.pe(fm_mm(pb2, csw + mp * 128), reads=[hk] + wkeys, writes=[pk2])
                r1k, r1 = r1rr.next()
                r2k, r2 = r2rr.next()
                P.dve(lambda e, r1=r1, pb1=pb1, c0=c0: e.tensor_tensor(out=r1[:], in0=pb1[:], in1=cosT[:, c0:c0 + 512], op=ALU.mult), reads=[pk1, "cosT"], writes=[r1k])
                P.dve(lambda e, r2=r2, pb2=pb2, c0=c0: e.tensor_tensor(out=r2[:], in0=pb2[:], in1=sinT[:, c0:c0 + 512], op=ALU.mult), reads=[pk2, "sinT"], writes=[r2k])
                qk_, qr = qrrr.next()
                P.pool(lambda e, qr=qr, r1=r1, r2=r2: e.tensor_tensor(out=qr[:], in0=r1[:], in1=r2[:], op=ALU.add), reads=[r1k, r2k], writes=[qk_])
                ok = "o%s%d_%d" % (which, g, mp)
                P.dma(qk_, lambda e, qr=qr, out_d=out_d, mp=mp, c0=c0: e.dma_start(out=out_d[mp * 128:(mp + 1) * 128, c0:c0 + 512], in_=qr[:]), reads=[qk_], writes=[ok])
                outs.append(ok)
        for hc in range(2):
            pk1, pb1 = prr.next()
            P.pe(fm_mm(pb1, C_HQ + hc * 128), reads=[hk] + wkeys, writes=[pk1])
            fk, fm = fmrr.next()
            P.dve(lambda e, fm=fm, pb1=pb1: e.tensor_copy(out=fm[:], in_=pb1[:]), reads=[pk1], writes=[fk])
            ok = "ohq%d_%d" % (g, hc)
            P.dma(fk, lambda e, fm=fm, hc=hc, c0=c0: e.dma_start(out=HQT_d[hc * 128:(hc + 1) * 128, c0:c0 + 512], in_=fm[:]), reads=[fk], writes=[ok])
            outs.append(ok)
            pk2, pb2 = prr.next()
            P.pe(fm_mm(pb2, C_HF + hc * 128), reads=[hk] + wkeys, writes=[pk2])
            fk, fm = fmrr.next()
            P.act(lambda e, fm=fm, pb2=pb2: e.activation(out=fm[:], in_=pb2[:], func=AF.Sigmoid, scale=-1.0), reads=[pk2], writes=[fk])
            ok = "osg%d_%d" % (g, hc)
            P.dma(fk, lambda e, fm=fm, hc=hc, c0=c0: e.dma_start(out=SGT_d[hc * 128:(hc + 1) * 128, c0:c0 + 512], in_=fm[:]), reads=[fk], writes=[ok])
            outs.append(ok)
    P.op("sp", None, reads=outs)
    return P.emit()


def build_p2(S, do_attn=True, do_hgrn=True, layer=0):
    P = Prog()
    NB = S // 128
    NQ = S // 512
    QT_d = P.dram("QTn", [64, S], BF16, "ExternalInput")
    KT_d = P.dram("KTn", [64, S], BF16, "ExternalInput")
    V_d = P.dram("Vh", [S, 128], BF16, "ExternalInput")
    OA_d = P.dram("OATT", [S, 128], F32, "ExternalOutput")
    HQ_d = P.dram("HQTh", [64, S], F32, "ExternalInput")
    SG_d = P.dram("SGTh", [64, S], F32, "ExternalInput")
    LF_d = P.dram("LOGFh", [S, 64], F32, "ExternalInput")
    VA_d = P.dram("VALhz", [S, 32], BF16, "ExternalInput")
    LB_d = P.dram("lblh", [64, 2], F32, "ExternalInput")
    TR_d = P.dram("TRIC", [64, 68], F32, "ExternalInput")
    OH_d = P.dram("OHG", [S, 32], F32, "ExternalOutput")
    outs = []

    st = [("st%d" % i, P.ps("st%d" % i, [128, 512], F32)) for i in range(3)]
    oacc = [("oa%d" % i, P.ps("oa%d" % i, [128, 2, 256], F32)) for i in range(4)]
    ptp = P.ps("ptp", [128, 1024], BF16)
    ident, identf = make_ident(P)

    if do_attn:
        QT = P.sb("QT", [64, S], BF16)
        KT = P.sb("KT", [64, S], BF16)
        V1 = P.sb("V1", [128, NB, 129], BF16)
        nsp = max(1, S // 4096)
        for i in range(nsp):
            a, b = i * (S // nsp), (i + 1) * (S // nsp)
            P.dma("QT%d" % i, lambda e, a=a, b=b: e.dma_start(out=QT[:, a:b], in_=QT_d[:, a:b]), writes=["QT%d" % i])
            P.dma("KT%d" % i, lambda e, a=a, b=b: e.dma_start(out=KT[:, a:b], in_=KT_d[:, a:b]), writes=["KT%d" % i])
            ba, bb = a // 128, b // 128
            P.dma("V1%d" % i, lambda e, a=a, b=b, ba=ba, bb=bb: e.dma_start(out=V1[:, ba:bb, 0:128], in_=V_d[a:b, :].rearrange("(b p) d -> p b d", p=128)),
                  writes=["V1%d" % i])
            P.pool(lambda e, ba=ba, bb=bb: e.memset(V1[:, ba:bb, 128:129], 1.0), writes=["V1o%d" % i])

        def seg(tok):
            return min(tok // (S // nsp), nsp - 1)
        trif = P.sb("trif", [128, 128], F32)
        tri = P.sb("tri", [128, 128], BF16)

        def mk(e):
            a = e.memset(trif[:], 1.0)
            b = e.affine_select(out=trif[:], in_=trif[:], pattern=[[1, 128]], compare_op=ALU.is_ge, fill=0.0, base=0, channel_multiplier=-1)
            return [a, b]
        P.pool(mk, writes=["trif"])
        P.dve(lambda e: e.tensor_copy(out=tri[:], in_=trif[:]), reads=["trif"], writes=["tri"])
        strr = RR(st)
        ptrr = RR([("pt%d" % i, P.sb("pt%d" % i, [128, 512], BF16)) for i in range(4)])
        osrr = RR([("os%d" % i, P.sb("os%d" % i, [128, 128], F32)) for i in range(4)])
        rcrr = RR([("rc%d" % i, P.sb("rc%d" % i, [128, 1], F32)) for i in range(4)])
        its = [(qt, kb) for qt in range(NQ) for kb in range(4 * qt + 4)]

        def acc(qt, c):
            t = oacc[(qt % 2) * 2 + c // 2]
            return t[0], t[1][:, c % 2, 0:129]
        pend = []

        def emit_st(qt, kb):
            j = kb - 4 * qt
            qoff = 0 if j < 0 else 128 * j
            sk, stt = strr.next()
            q0 = qt * 512 + qoff
            P.pe(lambda e: e.matmul(stt[:, qoff:512], KT[:, kb * 128:(kb + 1) * 128], QT[:, q0:(qt + 1) * 512], start=True, stop=True),
                 reads=["KT%d" % seg(kb * 128), "QT%d" % seg(q0)], writes=[sk])
            pk, ptt = ptrr.next()
            P.act(lambda e: e.activation(out=ptt[:, qoff:512], in_=stt[:, qoff:512], func=AF.Exp, scale=0.125), reads=[sk], writes=[pk])
            if j >= 0:
                P.pool(lambda e: e.tensor_tensor(out=ptt[:, qoff:qoff + 128], in0=ptt[:, qoff:qoff + 128], in1=tri[:], op=ALU.mult),
                       reads=[pk, "tri"], writes=[pk])
            return (qt, kb, pk, ptt, j)

        def emit_pv(item):
            qt, kb, pk, ptt, j = item
            for c in range(max(j, 0), 4):
                ak, av = acc(qt, c)
                P.pe(lambda e, av=av, c=c: e.matmul(av, ptt[:, c * 128:(c + 1) * 128], V1[:, kb, :], start=(kb == 0 and c % 2 == 0), stop=(kb == 4 * qt + c), skip_group_check=True),
                     reads=[pk, "V1%d" % seg(kb * 128), "V1o%d" % seg(kb * 128)], writes=[ak])
            if kb == 4 * qt + 3:
                for c in range(4):
                    ak, av = acc(qt, c)
                    rk, rc = rcrr.next()
                    ok, osb = osrr.next()
                    P.dve(lambda e, rc=rc, av=av: e.reciprocal(out=rc[:], in_=av[:, 128:129]), reads=[ak], writes=[rk])
                    P.dve(lambda e, rc=rc, av=av, osb=osb: e.tensor_scalar(out=osb[:], in0=av[:, 0:128], scalar1=rc[:, 0:1], scalar2=None, op0=ALU.mult),
                          reads=[ak, rk], writes=[ok])
                    r0 = (qt * 4 + c) * 128
                    P.dma(ok, lambda e, osb=osb, r0=r0: e.dma_start(out=OA_d[r0:r0 + 128, :], in_=osb[:]), reads=[ok], writes=["oOA%d" % r0])
                    outs.append("oOA%d" % r0)
        LOOK = 2
        for i, (qt, kb) in enumerate(its):
            pend.append(emit_st(qt, kb))
            if len(pend) > LOOK:
                emit_pv(pend.pop(0))
        while pend:
            emit_pv(pend.pop(0))

    if do_hgrn:
        NBAT = S // 512
        tr = P.sb("tr", [64, 68], F32)
        P.dma("tr", lambda e: e.dma_start(out=tr[:], in_=TR_d), writes=["tr"])
        lbt = P.sb("lbt", [64, 2], F32)
        oml = P.sb("oml", [64, 1], F32)
        if layer == 0:
            P.pool(lambda e: e.memset(oml[:], 1.0), writes=["oml"])
        else:
            P.dma("lbt", lambda e: e.dma_start(out=lbt[:], in_=LB_d), writes=["lbt"])
            P.dve(lambda e: e.tensor_tensor(out=oml[:], in0=lbt[:, 0:1], in1=lbt[:, 1:2], op=ALU.subtract), reads=["lbt"], writes=["oml"])
            P.act(lambda e: e.activation(out=oml[:], in_=oml[:], func=AF.Sigmoid), reads=["oml"], writes=["oml"])
        mk8f = P.sb("mk8f", [64, 8, 64], F32)

        def mkm(e):
            a = e.memset(mk8f[:], 1.0)
            b = e.affine_select(out=mk8f[:], in_=mk8f[:], pattern=[[0, 8], [1, 64]], compare_op=ALU.is_ge, fill=0.0, base=0, channel_multiplier=-1)
            return [a, b]
        P.pool(mkm, writes=["mk8f"])
        state = P.sb("state", [64, 32], F32)
        P.pool(lambda e: e.memset(state[:], 0.0), writes=["state"])
        smrr = RR([("sm%d" % i, P.sb("sm%d" % i, [64, 32], BF16)) for i in range(3)])
        lfrr = RR([("hlf%d" % i, P.sb("hlf%d" % i, [64, 8, 64], F32)) for i in range(2)])
        hqrr = RR([("hhq%d" % i, P.sb("hhq%d" % i, [64, 512], F32)) for i in range(2)])
        sgrr = RR([("hsg%d" % i, P.sb("hsg%d" % i, [64, 512], F32)) for i in range(2)])
        varr = RR([("hva%d" % i, P.sb("hva%d" % i, [64, 8, 32], BF16)) for i in range(2)])
        e1rr = RR([("he1%d" % i, P.sb("he1%d" % i, [64, 512], F32)) for i in range(2)])
        e2rr = RR([("he2%d" % i, P.sb("he2%d" % i, [64, 512], F32)) for i in range(2)])
        exrr = RR([("hex%d" % i, P.sb("hex%d" % i, [64, 8, 4], F32)) for i in range(2)])
        qdrr = RR([("hqd%d" % i, P.sb("hqd%d" % i, [64, 512], BF16)) for i in range(2)])
        kdrr = RR([("hkd%d" % i, P.sb("hkd%d" % i, [64, 512], BF16)) for i in range(2)])
        ktrr = RR([("hkt%d" % i, P.sb("hkt%d" % i, [64, 512], BF16)) for i in range(2)])
        atrr = RR([("hat%d" % i, P.sb("hat%d" % i, [64, 512], BF16)) for i in range(2)])
        usrr = RR([("hus%d" % i, P.sb("hus%d" % i, [64, 8, 32], F32)) for i in range(2)])
        oorr = RR([("hoo%d" % i, P.sb("hoo%d" % i, [64, 8, 32], F32)) for i in range(2)])
        bcrr = RR(st[0:2])
        scrr = RR([oacc[0]])
        oprr = RR([oacc[2], oacc[3]])
        exps = RR([("st2", st[2][1][0:64, 0:32])])
        for b in range(NBAT):
            t0 = b * 512
            lk, lf = lfrr.next()
            qk, hq = hqrr.next()
            sk, sg = sgrr.next()
            vk, va = varr.next()
            P.dma(lk, lambda e, lf=lf, t0=t0: e.dma_start(out=lf[:], in_=LF_d[t0:t0 + 512, :].rearrange("(c s) k -> s c k", s=64)), writes=[lk])
            P.dma(qk, lambda e, hq=hq, t0=t0: e.dma_start(out=hq[:], in_=HQ_d[:, t0:t0 + 512]), writes=[qk])
            P.dma(sk, lambda e, sg=sg, t0=t0: e.dma_start(out=sg[:], in_=SG_d[:, t0:t0 + 512]), writes=[sk])
            P.dma(vk, lambda e, va=va, t0=t0: e.dma_start(out=va[:], in_=VA_d[t0:t0 + 512, :].rearrange("(c s) v -> s c v", s=64)), writes=[vk])
            bk, bc = bcrr.next()
            xk, xp = exps.next()
            P.pe(lambda e, bc=bc, lf=lf: [e.matmul(bc[0:64, c * 64:(c + 1) * 64], lf[:, c, :], tr[:, 0:64], start=True, stop=True) for c in range(8)],
                 reads=[lk, "tr"], writes=[bk])
            P.pe(lambda e, xp=xp, lf=lf: [e.matmul(xp[:, c * 4:(c + 1) * 4], lf[:, c, :], tr[:, 64:68], start=True, stop=True) for c in range(8)],
                 reads=[lk, "tr"], writes=[xk])
            e1k, e1 = e1rr.next()
            e2k, e2 = e2rr.next()
            exk, ex = exrr.next()
            P.act(lambda e, e1=e1, bc=bc: e.activation(out=e1[:], in_=bc[0:64, :], func=AF.Exp), reads=[bk], writes=[e1k])
            P.act(lambda e, e2=e2, bc=bc: e.activation(out=e2[:], in_=bc[0:64, :], func=AF.Exp, scale=-1.0), reads=[bk], writes=[e2k])
            P.act(lambda e, ex=ex, xp=xp: e.activation(out=ex[:].rearrange("p c f -> p (c f)"), in_=xp, func=AF.Exp), reads=[xk], writes=[exk])
            qdk, qd = qdrr.next()
            kdk, kd = kdrr.next()
            P.dve(lambda e, qd=qd, hq=hq, e1=e1: e.tensor_tensor(out=qd[:], in0=hq[:], in1=e1[:], op=ALU.mult), reads=[qk, e1k], writes=[qdk])
            P.dve(lambda e, kd=kd, sg=sg, e2=e2: e.scalar_tensor_tensor(out=kd[:], in0=sg[:], scalar=oml[:, 0:1], in1=e2[:], op0=ALU.mult, op1=ALU.mult),
                   reads=[sk, e2k, "oml"], writes=[kdk])
            P.pe(lambda e, kd=kd: [e.transpose(ptp[0:64, c * 64:(c + 1) * 64], kd[:, c * 64:(c + 1) * 64], ident[0:64, 0:64]) for c in range(8)],
                 reads=[kdk, "ident"], writes=["ptp"])
            ktk, kt = ktrr.next()
            P.act(lambda e, kt=kt: e.copy(out=kt[:], in_=ptp[0:64, 0:512]), reads=["ptp"], writes=[ktk])
            sck, sc = scrr.next()
            scv = sc[0:64, :, :].rearrange("p a b -> p (a b)")
            P.pe(lambda e, scv=scv, kd=kd, qd=qd: [e.matmul(scv[:, c * 64:(c + 1) * 64], kd[:, c * 64:(c + 1) * 64], qd[:, c * 64:(c + 1) * 64], start=True, stop=True) for c in range(8)],
                 reads=[kdk, qdk], writes=[sck])
            atk, at = atrr.next()
            P.dve(lambda e, at=at, scv=scv: e.tensor_tensor(out=at[:], in0=scv, in1=mk8f[:].rearrange("p c t -> p (c t)"), op=ALU.mult),
                  reads=[sck, "mk8f"], writes=[atk])
            uk_ = "oa1"
            upv = oacc[1][1][0:64, 0, :]
            mk_, mi = oprr.next()
            opv = mi[0:64, 0, :]
            P.pe(lambda e, upv=upv, kt=kt, va=va: [e.matmul(upv[:, c * 32:(c + 1) * 32], kt[:, c * 64:(c + 1) * 64], va[:, c, :], start=True, stop=True) for c in range(8)],
                 reads=[ktk, vk], writes=[uk_])
            usk, us = usrr.next()
            P.dve(lambda e, us=us, upv=upv, ex=ex: e.tensor_tensor(out=us[:], in0=upv.rearrange("p (c v) -> p c v", c=8), in1=ex[:, :, 2:3].to_broadcast([64, 8, 32]), op=ALU.mult),
                  reads=[uk_, exk], writes=[usk])
            for c in range(8):
                smk, sm = smrr.next()
                P.dve(lambda e, sm=sm, ex=ex, c=c: e.tensor_scalar(out=sm[:], in0=state[:], scalar1=ex[:, c, 0:1], scalar2=None, op0=ALU.mult),
                      reads=["state", exk], writes=[smk])
                P.pe(lambda e, opv=opv, at=at, va=va, qd=qd, sm=sm, c=c: [
                    e.matmul(opv[:, c * 32:(c + 1) * 32], at[:, c * 64:(c + 1) * 64], va[:, c, :], start=True, stop=False),
                    e.matmul(opv[:, c * 32:(c + 1) * 32], qd[:, c * 64:(c + 1) * 64], sm[:], start=False, stop=True)],
                    reads=[atk, vk, qdk, smk], writes=[mk_])
                P.dve(lambda e, ex=ex, us=us, c=c: e.scalar_tensor_tensor(out=state[:], in0=state[:], scalar=ex[:, c, 1:2], in1=us[:, c, :], op0=ALU.mult, op1=ALU.add),
                      reads=["state", exk, usk], writes=["state"])
            ook, oo = oorr.next()
            P.act(lambda e, oo=oo, opv=opv: e.copy(out=oo[:].rearrange("p c v -> p (c v)"), in_=opv), reads=[mk_], writes=[ook])
            P.dma(ook, lambda e, oo=oo, t0=t0: e.dma_start(out=OH_d[t0:t0 + 512, :].rearrange("(c t) v -> t c v", t=64), in_=oo[:]), reads=[ook], writes=["oOH%d" % b])
            outs.append("oOH%d" % b)
    P.op("sp", None, reads=outs)
    return P.emit()

import math

D = 1024
ALPHA = 4 ** 0.25
EPS = 1e-5


def build_p3(T, layer, F, E, NJ):
    P = Prog()
    NT = T // 128
    G = T // 512
    moe = E > 1
    lam_init = 0.8 - 0.6 * math.exp(-0.3 * layer)
    x_d = P.dram("x", [T, D], F32, "ExternalInput")
    oa_d = P.dram("OATT", [T, 1024], F32, "ExternalInput")
    og_d = P.dram("OHG", [T, 256], F32, "ExternalInput")
    sg_d = P.dram("SG", [T, 256], F32, "ExternalInput")
    uh_d = P.dram("UH", [T + 128, 256], F32, "ExternalInput")
    ct_d = P.dram("ct", [128, 8], F32, "ExternalInput")
    wada_d = P.dram("wada", [D, 4096], F32, "ExternalInput")
    bada_d = P.dram("bada", [4096], F32, "ExternalInput")
    lq_d = P.dram("lamqk", [256], F32, "ExternalInput")
    ag_d = P.dram("attng", [128], F32, "ExternalInput")
    hgg_d = P.dram("hgg", [64], F32, "ExternalInput")
    pw_d = P.dram("poolw", [64, 4, 64], F32, "ExternalInput")
    psc_d = P.dram("pscale", [256], F32, "ExternalInput")
    wout_d = P.dram("wout", [D, D], F32, "ExternalInput")
    ln_d = P.dram("lnp", [4, D], F32, "ExternalInput")
    mt_d = P.dram("MT", [128, 4, 128], F32, "ExternalInput")
    mt0_d = P.dram("MT0", [128, 4, 128], F32, "ExternalInput")
    mtp_d = P.dram("MTP", [128, 4, 128], F32, "ExternalInput")
    w1_d = P.dram("w1", [E, D, F], F32, "ExternalInput")
    w3_d = P.dram("w3", [E, D, F], F32, "ExternalInput")
    w2_d = P.dram("w2", [E, F, D], F32, "ExternalInput")
    if moe:
        rw_d = P.dram("rw", [128, 8, 8], F32, "ExternalInput")
    x1_d = P.dram("x1s", [T, D], F32, "Internal")
    xo_d = P.dram("xo", [T, D], F32, "ExternalOutput")
    outs = []

    pbs = [("pb%d" % i, P.ps("pb%d" % i, [128, 512], F32)) for i in range(7)]
    prr = RR(pbs)
    psT = P.ps("psT", [128, 1024], BF16)
    ident, identf = make_ident(P)
    epst = P.sb("epst", [128, 1], F32)
    P.pool(lambda e: e.memset(epst[:], EPS), writes=["epst"])

    h2T = P.sb("h2T", [128, 8, T], BF16)
    modg2 = P.sb("modg2", [128, 1024], F32)
    lnb2 = P.sb("lnb2", [128, 2, D], F32)
    if moe:
        comb = P.sb("comb", [128, NT, 8], F32)

    P.open_scope()
    mod = P.sb("mod", [128, 3072], F32)
    lnb1 = P.sb("lnb1", [128, 2, D], F32)
    ls = P.sb("ls", [128, 2], F32)
    nlam = P.sb("nlam", [128, 1], F32)
    gA = P.sb("gA", [128, 128], F32)
    hgG = P.sb("hgG", [128, 64], F32)
    pscb = P.sb("pscb", [128, 256], F32)
    pw = P.sb("pw", [64, 4, 64], F32)
    mt = P.sb("mt", [128, 4, 128], F32)
    mt0 = P.sb("mt0", [128, 4, 128], F32)
    mtp = P.sb("mtp", [128, 4, 128], F32)
    scl8 = P.sb("scl8", [128, 8], F32)
    wob = P.sb("wob", [128, 8, D], BF16)
    if moe:
        rw = P.sb("rw_s", [128, 8, 8], F32)
    P.open_scope()
    ct = P.sb("ct_s", [128, 8], F32)
    cond = P.sb("cond", [128, 8], F32)
    condb = P.sb("condb", [128, 8, 128], F32)
    P.dma("ct", lambda e: e.dma_start(out=ct[:], in_=ct_d), writes=["ct"])
    P.act(lambda e: e.activation(out=cond[:], in_=ct[:], func=AF.Silu), reads=["ct"], writes=["cond"])
    P.dve(lambda e: e.tensor_copy(out=condb[:], in_=cond[:].unsqueeze(2).to_broadcast([128, 8, 128])), reads=["cond"], writes=["condb"])
    badab = P.sb("badab", [128, 4096], F32)
    P.dma("badab", lambda e: e.dma_start(out=badab[:], in_=bada_d.partition_broadcast(128)), writes=["badab"])
    wrr = RR([("wst%d" % i, P.sb("wst%d" % i, [128, 512], F32)) for i in range(3)])
    for n in range(8):
        pk, pb = prr.next()
        for k in range(8):
            wk, wt = wrr.next()
            P.dma(wk, lambda e, wt=wt, k=k, n=n: e.dma_start(out=wt[:], in_=wada_d[k * 128:(k + 1) * 128, n * 512:(n + 1) * 512]), writes=[wk])
            P.pe(lambda e, pb=pb, wt=wt, k=k: e.matmul(pb[:], condb[:, k, :], wt[:], start=(k == 0), stop=(k == 7)), reads=[wk, "condb"], writes=[pk])
        dst = mod[:, n * 512:(n + 1) * 512] if n < 6 else modg2[:, (n - 6) * 512:(n - 5) * 512]
        P.dve(lambda e, pb=pb, n=n, dst=dst: e.tensor_tensor(out=dst, in0=pb[:], in1=badab[:, n * 512:(n + 1) * 512], op=ALU.add),
              reads=[pk, "badab"], writes=["mod" if n < 6 else "modg2"])
    P.dve(lambda e: e.tensor_scalar_add(out=mod[:, 0:1024], in0=mod[:, 0:1024], scalar1=1.0), reads=["mod"], writes=["mod"])
    P.dve(lambda e: e.tensor_scalar_add(out=mod[:, 2048:3072], in0=mod[:, 2048:3072], scalar1=1.0), reads=["mod"], writes=["mod"])
    P.dve(lambda e: e.tensor_scalar_add(out=modg2[:], in0=modg2[:], scalar1=1.0), reads=["modg2"], writes=["modg2"])
    OPG1, SH2, OPSC2, OPG2 = mod[:, 0:1024], mod[:, 1024:2048], mod[:, 2048:3072], modg2[:]
    P.dma("lnb1", lambda e: e.dma_start(out=lnb1[:].rearrange("p a d -> p (a d)"), in_=ln_d[0:2, :].rearrange("a d -> (a d)").partition_broadcast(128)), writes=["lnb1"])
    P.dma("lnb2", lambda e: e.dma_start(out=lnb2[:].rearrange("p a d -> p (a d)"), in_=ln_d[2:4, :].rearrange("a d -> (a d)").partition_broadcast(128)), writes=["lnb2"])
    lqb = P.sb("lqb", [128, 256], F32)
    lpr = P.sb("lpr", [128, 256], F32)
    P.dma("lqb", lambda e: e.dma_start(out=lqb[:], in_=lq_d.partition_broadcast(128)), writes=["lqb"])
    lq4 = lqb[:].rearrange("p (a b d) -> p a b d", a=2, b=2)
    P.dve(lambda e: e.tensor_tensor(out=lpr[:, 0:128].rearrange("p (a d) -> p a d", a=2), in0=lq4[:, :, 0, :], in1=lq4[:, :, 1, :], op=ALU.mult), reads=["lqb"], writes=["lpr"])
    P.dve(lambda e: e.tensor_reduce(out=ls[:], in_=lpr[:, 0:128].rearrange("p (a d) -> p a d", a=2), axis=AX.X, op=ALU.add), reads=["lpr"], writes=["ls"])
    P.act(lambda e: e.activation(out=ls[:], in_=ls[:], func=AF.Exp), reads=["ls"], writes=["ls"])
    P.dve(lambda e: e.tensor_tensor(out=nlam[:], in0=ls[:, 1:2], in1=ls[:, 0:1], op=ALU.subtract), reads=["ls"], writes=["nlam"])
    P.dve(lambda e: e.tensor_scalar_add(out=nlam[:], in0=nlam[:], scalar1=-lam_init), reads=["nlam"], writes=["nlam"])
    P.dma("gA", lambda e: e.dma_start(out=gA[:], in_=ag_d.partition_broadcast(128)), writes=["gA"])
    P.dve(lambda e: e.tensor_scalar_mul(out=gA[:], in0=gA[:], scalar1=1.0 - lam_init), reads=["gA"], writes=["gA"])
    P.dma("hgG", lambda e: e.dma_start(out=hgG[:], in_=hgg_d.partition_broadcast(128)), writes=["hgG"])
    P.dma("pscb", lambda e: e.dma_start(out=pscb[:], in_=psc_d.partition_broadcast(128)), writes=["pscb"])
    P.dma("pw", lambda e: e.dma_start(out=pw[:], in_=pw_d), writes=["pw"])
    P.dma("mt", lambda e: e.dma_start(out=mt[:], in_=mt_d), writes=["mt"])
    P.dma("mt0", lambda e: e.dma_start(out=mt0[:], in_=mt0_d), writes=["mt0"])
    P.dma("mtp", lambda e: e.dma_start(out=mtp[:], in_=mtp_d), writes=["mtp"])
    P.pool(lambda e: [e.memset(scl8[:, 0:4], 1.0 / 128), e.memset(scl8[:, 4:8], 1.0 / 64)], writes=["scl8"])
    for j in range(8):
        P.dma("wob%d" % j, lambda e, j=j: e.dma_start(out=wob[:, j, :], in_=wout_d[j * 128:(j + 1) * 128, :]), writes=["wob%d" % j], eng="pool")
    wobk = ["wob%d" % j for j in range(8)]
    if moe:
        P.dma("rw", lambda e: e.dma_start(out=rw[:], in_=rw_d), writes=["rw"])

    P.close_scope()
    def rr(name, shape, dt, n=2):
        return RR([("%s%d" % (name, i), P.sb("%s%d" % (name, i), shape, dt)) for i in range(n)])
    xrr = rr("xt", [128, D], F32)
    oarr = rr("oat", [128, 1024], F32)
    ogrr = rr("ogt", [128, 256], F32)
    sgrr = rr("sgt", [128, 256], F32)
    ucrr = rr("uct", [128, 256], F32)
    uprr = rr("upt", [128, 256], F32)
    afrr = rr("af", [128, 512], F32)
    sqrr = rr("sq", [128, 512], F32)
    ssrr = rr("ss8", [128, 8], F32)
    rsrr = rr("rs8", [128, 8], F32)
    trr_ = rr("trr", [128, 256], F32)
    mixrr = rr("mix", [128, D], BF16)
    mxtrr = rr("mixT", [128, 8, 128], BF16)
    ptrr = rr("pT", [64, 512], F32)
    t1rr = rr("t1", [128, D], F32)
    y1rr = rr("y1", [128, D], F32)
    strr = rr("bst", [128, 2, 6], F32)
    mvrr = rr("mv", [128, 2], F32)
    rdrr = rr("rstd", [128, 1], F32)
    x1rr = rr("x1t", [128, D], F32)
    hfrr = rr("h2f", [128, D], F32)
    hbrr = rr("h2b", [128, D], BF16)
    if moe:
        hftrr = rr("h2fT", [128, 8, 128], F32)
        lgrr = rr("lg", [128, 8], F32)
        l2rr = rr("lg2", [128, 8], F32)
        m1rr = rr("m1", [128, 4], F32)
    for i in range(NT):
        r0 = i * 128
        xk, xt = xrr.next()
        oak, oat = oarr.next()
        ogk, ogt = ogrr.next()
        sgk, sgt = sgrr.next()
        uck, uct = ucrr.next()
        upk, upt = uprr.next()
        P.dma(xk, lambda e, xt=xt, r0=r0: e.dma_start(out=xt[:], in_=x_d[r0:r0 + 128, :]), writes=[xk])
        P.dma(oak, lambda e, oat=oat, r0=r0: e.dma_start(out=oat[:], in_=oa_d[r0:r0 + 128, :]), writes=[oak])
        P.dma(ogk, lambda e, ogt=ogt, r0=r0: e.dma_start(out=ogt[:], in_=og_d[r0:r0 + 128, :]), writes=[ogk])
        P.dma(sgk, lambda e, sgt=sgt, r0=r0: e.dma_start(out=sgt[:], in_=sg_d[r0:r0 + 128, :]), writes=[sgk])
        P.dma(uck, lambda e, uct=uct, r0=r0: e.dma_start(out=uct[:], in_=uh_d[r0 + 128:r0 + 256, :]), writes=[uck])
        P.dma(upk, lambda e, upt=upt, r0=r0: e.dma_start(out=upt[:], in_=uh_d[r0:r0 + 128, :]), writes=[upk])
        afk, af = afrr.next()
        sqk, sq = sqrr.next()
        ssk, ss8 = ssrr.next()
        rsk, rs8 = rsrr.next()
        trk, trt = trr_.next()
        mk, mix = mixrr.next()
        oa4 = oat[:].rearrange("p (h m d) -> p h m d", h=4, m=2)
        af3 = af[:].rearrange("p (h d) -> p h d", h=4)
        sq3 = sq[:].rearrange("p (h d) -> p h d", h=4)
        og3 = ogt[:].rearrange("p (h d) -> p h d", h=4)
        tr3 = trt[:].rearrange("p (h d) -> p h d", h=4)
        P.dve(lambda e, af3=af3, oa4=oa4: e.scalar_tensor_tensor(out=af3, in0=oa4[:, :, 1, :], scalar=nlam[:, 0:1], in1=oa4[:, :, 0, :], op0=ALU.mult, op1=ALU.add),
              reads=[oak, "nlam"], writes=[afk])
        P.pool(lambda e, sq=sq, af=af: e.tensor_tensor(out=sq[:], in0=af[:], in1=af[:], op=ALU.mult), reads=[afk], writes=[sqk])
        P.dve(lambda e, ss8=ss8, sq3=sq3: e.tensor_reduce(out=ss8[:, 0:4], in_=sq3, axis=AX.X, op=ALU.add), reads=[sqk], writes=[ssk])
        P.pool(lambda e, sq=sq, ogt=ogt: e.tensor_tensor(out=sq[:, 0:256], in0=ogt[:], in1=ogt[:], op=ALU.mult), reads=[ogk, ssk], writes=[sqk])
        P.dve(lambda e, ss8=ss8, sq=sq: e.tensor_reduce(out=ss8[:, 4:8], in_=sq[:, 0:256].rearrange("p (h d) -> p h d", h=4), axis=AX.X, op=ALU.add), reads=[sqk], writes=[ssk])
        P.pool(lambda e, ss8=ss8: e.tensor_tensor(out=ss8[:], in0=ss8[:], in1=scl8[:], op=ALU.mult), reads=[ssk, "scl8"], writes=[ssk])
        P.act(lambda e, rs8=rs8, ss8=ss8: e.activation(out=rs8[:], in_=ss8[:], func=AF.Sqrt, bias=epst[:], scale=1.0), reads=[ssk, "epst"], writes=[rsk])
        P.dve(lambda e, rs8=rs8: e.reciprocal(out=rs8[:], in_=rs8[:]), reads=[rsk], writes=[rsk])
        P.dve(lambda e, af3=af3, rs8=rs8: e.tensor_tensor(out=af3, in0=af3, in1=rs8[:, 0:4].unsqueeze(2).to_broadcast([128, 4, 128]), op=ALU.mult), reads=[afk, rsk], writes=[afk])
        P.pool(lambda e, mix=mix, af3=af3: e.tensor_tensor(out=mix[:, 0:512].rearrange("p (h d) -> p h d", h=4), in0=af3, in1=gA[:].unsqueeze(1).to_broadcast([128, 4, 128]), op=ALU.mult),
               reads=[afk, "gA"], writes=[mk])
        P.dve(lambda e, tr3=tr3, og3=og3, rs8=rs8: e.tensor_tensor(out=tr3, in0=og3, in1=rs8[:, 4:8].unsqueeze(2).to_broadcast([128, 4, 64]), op=ALU.mult), reads=[ogk, rsk], writes=[trk])
        P.pool(lambda e, trt=trt, sgt=sgt: e.tensor_tensor(out=trt[:], in0=trt[:], in1=sgt[:], op=ALU.mult), reads=[trk, sgk], writes=[trk])
        P.pool(lambda e, mix=mix, tr3=tr3: e.tensor_tensor(out=mix[:, 512:768].rearrange("p (h d) -> p h d", h=4), in0=tr3, in1=hgG[:].unsqueeze(1).to_broadcast([128, 4, 64]), op=ALU.mult),
               reads=[trk, "hgG"], writes=[mk])
        pk1, pb1 = prr.next()
        mtc = mt0 if i == 0 else mt
        mtck = "mt0" if i == 0 else "mt"
        P.pe(lambda e, pb1=pb1, uct=uct, upt=upt, mtc=mtc: sum([[
            e.matmul(pb1[0:64, g * 128:(g + 1) * 128], uct[:, g * 64:(g + 1) * 64], mtc[:, g, :], start=True, stop=False),
            e.matmul(pb1[0:64, g * 128:(g + 1) * 128], upt[:, g * 64:(g + 1) * 64], mtp[:, g, :], start=False, stop=True)] for g in range(4)], []),
            reads=[uck, upk, mtck, "mtp"], writes=[pk1])
        ptk, pT = ptrr.next()
        P.act(lambda e, pT=pT, pb1=pb1: e.copy(out=pT[:], in_=pb1[0:64, :]), reads=[pk1], writes=[ptk])
        pk2, pb2 = prr.next()
        P.pe(lambda e, pb2=pb2, pT=pT: [e.matmul(pb2[:, g * 64:(g + 1) * 64], pT[:, g * 128:(g + 1) * 128], pw[:, g, :], start=True, stop=True) for g in range(4)],
             reads=[ptk, "pw"], writes=[pk2])
        P.dve(lambda e, mix=mix, pb2=pb2: e.tensor_tensor(out=mix[:, 768:1024], in0=pb2[:, 0:256], in1=pscb[:], op=ALU.mult), reads=[pk2, "pscb"], writes=[mk])
        P.pe(lambda e, mix=mix: [e.transpose(psT[:, k * 128:(k + 1) * 128], mix[:, k * 128:(k + 1) * 128], ident[:]) for k in range(8)], reads=[mk, "ident"], writes=["psT"])
        mtk, mixT = mxtrr.next()
        P.act(lambda e, mixT=mixT: e.copy(out=mixT[:], in_=psT[:].rearrange("p (k t) -> p k t", k=8)), reads=["psT"], writes=[mtk])
        t1k, t1 = t1rr.next()
        for hh in range(2):
            pkm, pbm = prr.next()
            P.pe(lambda e, pbm=pbm, mixT=mixT, hh=hh: [e.matmul(pbm[:], mixT[:, k, :], wob[:, k, hh * 512:(hh + 1) * 512], start=(k == 0), stop=(k == 7)) for k in range(8)],
                 reads=[mtk] + wobk, writes=[pkm])
            P.dve(lambda e, t1=t1, pbm=pbm, hh=hh: e.tensor_tensor(out=t1[:, hh * 512:(hh + 1) * 512], in0=pbm[:], in1=OPG1[:, hh * 512:(hh + 1) * 512], op=ALU.mult),
                  reads=[pkm, "mod"], writes=[t1k])
        y1k, y1 = y1rr.next()
        P.dve(lambda e, y1=y1, xt=xt, t1=t1: e.scalar_tensor_tensor(out=y1[:], in0=xt[:], scalar=ALPHA, in1=t1[:], op0=ALU.mult, op1=ALU.add), reads=[xk, t1k], writes=[y1k])
        stk, bst = strr.next()
        mvk, mv = mvrr.next()
        rdk, rstd = rdrr.next()
        P.dve(lambda e, bst=bst, y1=y1: [e.bn_stats(out=bst[:, 0, :], in_=y1[:, 0:512]), e.bn_stats(out=bst[:, 1, :], in_=y1[:, 512:1024])], reads=[y1k], writes=[stk])
        P.dve(lambda e, mv=mv, bst=bst: e.bn_aggr(out=mv[:], in_=bst[:]), reads=[stk], writes=[mvk])
        P.act(lambda e, rstd=rstd, mv=mv: e.activation(out=rstd[:], in_=mv[:, 1:2], func=AF.Sqrt, bias=epst[:], scale=1.0), reads=[mvk, "epst"], writes=[rdk])
        P.dve(lambda e, rstd=rstd: e.reciprocal(out=rstd[:], in_=rstd[:]), reads=[rdk], writes=[rdk])
        x1k, x1t = x1rr.next()
        P.dve(lambda e, x1t=x1t, y1=y1, mv=mv, rstd=rstd: e.tensor_scalar(out=x1t[:], in0=y1[:], scalar1=mv[:, 0:1], scalar2=rstd[:, 0:1], op0=ALU.subtract, op1=ALU.mult),
              reads=[y1k, mvk, rdk], writes=[x1k])
        P.pool(lambda e, x1t=x1t: e.tensor_tensor(out=x1t[:], in0=x1t[:], in1=lnb1[:, 0, :], op=ALU.mult), reads=[x1k, "lnb1"], writes=[x1k])
        P.pool(lambda e, x1t=x1t: e.tensor_tensor(out=x1t[:], in0=x1t[:], in1=lnb1[:, 1, :], op=ALU.add), reads=[x1k, "lnb1"], writes=[x1k])
        P.dma(x1k, lambda e, x1t=x1t, r0=r0: e.dma_start(out=x1_d[r0:r0 + 128, :], in_=x1t[:]), reads=[x1k], writes=["x1d%d" % i])
        hfk, h2f = hfrr.next()
        hbk, h2b = hbrr.next()
        P.pool(lambda e, h2f=h2f, x1t=x1t: e.tensor_tensor(out=h2f[:], in0=x1t[:], in1=OPSC2, op=ALU.mult), reads=[x1k, "mod"], writes=[hfk])
        P.pool(lambda e, h2f=h2f: e.tensor_tensor(out=h2f[:], in0=h2f[:], in1=SH2, op=ALU.add), reads=[hfk, "mod"], writes=[hfk])
        P.act(lambda e, h2b=h2b, h2f=h2f: e.copy(out=h2b[:], in_=h2f[:]), reads=[hfk], writes=[hbk])
        P.pe(lambda e, h2b=h2b: [e.transpose(psT[:, k * 128:(k + 1) * 128], h2b[:, k * 128:(k + 1) * 128], ident[:]) for k in range(8)], reads=[hbk, "ident"], writes=["psT"])
        P.act(lambda e, r0=r0: e.copy(out=h2T[:, :, r0:r0 + 128], in_=psT[:].rearrange("p (k t) -> p k t", k=8)), reads=["psT"], writes=["h2T%d" % (i // 4)])
        if moe:
            hftk, h2fT = hftrr.next()
            for hh in range(2):
                pkt, pbt = prr.next()
                P.pe(lambda e, pbt=pbt, h2f=h2f, hh=hh: [e.transpose(pbt[:, kk * 128:(kk + 1) * 128], h2f[:, (hh * 4 + kk) * 128:(hh * 4 + kk + 1) * 128], identf[:]) for kk in range(4)],
                     reads=[hfk, "identf"], writes=[pkt])
                P.act(lambda e, h2fT=h2fT, pbt=pbt, hh=hh: e.copy(out=h2fT[:, hh * 4:(hh + 1) * 4, :], in_=pbt[:].rearrange("p (k t) -> p k t", k=4)), reads=[pkt], writes=[hftk])
            pkl, pbl = prr.next()
            P.pe(lambda e, pbl=pbl, h2fT=h2fT: [e.matmul(pbl[:, 0:8], h2fT[:, k, :], rw[:, k, :], start=(k == 0), stop=(k == 7)) for k in range(8)], reads=[hftk, "rw"], writes=[pkl])
            lgk, lg = lgrr.next()
            l2k, lg2 = l2rr.next()
            m1k, m1 = m1rr.next()
            P.dve(lambda e, lg=lg, pbl=pbl: e.tensor_copy(out=lg[:], in_=pbl[:, 0:8]), reads=[pkl], writes=[lgk])
            P.dve(lambda e, m1=m1, lg=lg: e.tensor_reduce(out=m1[:, 0:1], in_=lg[:], axis=AX.X, op=ALU.max), reads=[lgk], writes=[m1k])
            P.dve(lambda e, lg2=lg2, lg=lg, m1=m1: e.tensor_scalar(out=lg2[:], in0=lg[:], scalar1=m1[:, 0:1], scalar2=-1e30, op0=ALU.is_equal, op1=ALU.mult), reads=[lgk, m1k], writes=[l2k])
            P.dve(lambda e, lg2=lg2, lg=lg: e.tensor_tensor(out=lg2[:], in0=lg2[:], in1=lg[:], op=ALU.add), reads=[lgk, l2k], writes=[l2k])
            P.dve(lambda e, m1=m1, lg2=lg2: e.tensor_reduce(out=m1[:, 1:2], in_=lg2[:], axis=AX.X, op=ALU.max), reads=[l2k, m1k], writes=[m1k])
            P.dve(lambda e, lg2=lg2, lg=lg, m1=m1: e.tensor_scalar(out=lg2[:], in0=lg[:], scalar1=m1[:, 1:2], scalar2=None, op0=ALU.is_ge), reads=[lgk, m1k, l2k], writes=[l2k])
            P.dve(lambda e, m1=m1: e.tensor_scalar_mul(out=m1[:, 2:3], in0=m1[:, 0:1], scalar1=-1.0), reads=[m1k], writes=[m1k])
            P.act(lambda e, lg=lg, m1=m1: e.activation(out=lg[:], in_=lg[:], func=AF.Exp, bias=m1[:, 2:3], scale=1.0), reads=[lgk, m1k], writes=[lgk])
            P.dve(lambda e, lg=lg, lg2=lg2: e.tensor_tensor(out=lg[:], in0=lg[:], in1=lg2[:], op=ALU.mult), reads=[lgk, l2k], writes=[lgk])
            P.dve(lambda e, m1=m1, lg=lg: e.tensor_reduce(out=m1[:, 3:4], in_=lg[:], axis=AX.X, op=ALU.add), reads=[lgk, m1k], writes=[m1k])
            P.dve(lambda e, m1=m1: e.reciprocal(out=m1[:, 3:4], in_=m1[:, 3:4]), reads=[m1k], writes=[m1k])
            P.dve(lambda e, lg=lg, m1=m1, i=i: e.tensor_scalar(out=comb[:, i, :], in0=lg[:], scalar1=m1[:, 3:4], scalar2=None, op0=ALU.mult), reads=[lgk, m1k], writes=["comb"])
    P.close_scope()

    facc = P.sb("facc", [128, NT, D], F32)
    for i in range(NT):
        P.pool(lambda e, i=i: e.memset(facc[:, i, :], 0.0), writes=["facc%d" % i])
    HB = 128 * NJ
    NBLK = F // HB
    w1rr = rr("w1b", [128, 8, HB], BF16)
    w3rr = rr("w3b", [128, 8, HB], BF16)
    w2rr = rr("w2b", [128, NJ, D], BF16)
    srr = rr("sil", [128, 512], F32, 3)
    atrr = rr("actT", [128, NJ, 512], BF16)
    uprr_ = RR(pbs[0:4])
    dnrr = RR(pbs[4:7])
    pend = None

    def emit_down(item):
        ex, atk, actT, w2k, w2b, g = item
        for tt in range(4):
            ti = g * 4 + tt
            for dh in range(2):
                pkd, pbd = dnrr.next()
                P.pe(lambda e, pbd=pbd, tt=tt, dh=dh: [e.matmul(pbd[:], actT[:, j, tt * 128:(tt + 1) * 128], w2b[:, j, dh * 512:(dh + 1) * 512], start=(j == 0), stop=(j == NJ - 1)) for j in range(NJ)],
                     reads=[atk, w2k], writes=[pkd])
                fa = facc[:, ti, dh * 512:(dh + 1) * 512]
                if moe:
                    P.dve(lambda e, pbd=pbd, fa=fa, ti=ti: e.scalar_tensor_tensor(out=fa, in0=pbd[:], scalar=comb[:, ti, ex:ex + 1], in1=fa, op0=ALU.mult, op1=ALU.add),
                          reads=[pkd, "comb", "facc%d" % ti], writes=["facc%d" % ti])
                else:
                    P.dve(lambda e, pbd=pbd, fa=fa: e.tensor_tensor(out=fa, in0=pbd[:], in1=fa, op=ALU.add), reads=[pkd, "facc%d" % ti], writes=["facc%d" % ti])
    for ex in range(E):
        for hb in range(NBLK):
            c0 = hb * HB
            w1k, w1b = w1rr.next()
            w3k, w3b = w3rr.next()
            w2k, w2b = w2rr.next()
            P.dma(w1k, lambda e, w1b=w1b, ex=ex, c0=c0: e.dma_start(out=w1b[:], in_=w1_d[ex, :, c0:c0 + HB].rearrange("(k p) n -> p k n", p=128)), writes=[w1k], eng="pool")
            P.dma(w3k, lambda e, w3b=w3b, ex=ex, c0=c0: e.dma_start(out=w3b[:], in_=w3_d[ex, :, c0:c0 + HB].rearrange("(k p) n -> p k n", p=128)), writes=[w3k], eng="pool")
            P.dma(w2k, lambda e, w2b=w2b, ex=ex, c0=c0: e.dma_start(out=w2b[:], in_=w2_d[ex, c0:c0 + HB, :].rearrange("(j p) d -> p j d", p=128)), writes=[w2k], eng="pool")
            for g in range(G):
                atk, actT = atrr.next()
                for j in range(NJ):
                    pk1, pb1 = uprr_.next()
                    pk3, pb3 = uprr_.next()
                    P.pe(lambda e, pb1=pb1, w1b=w1b, j=j, g=g: [e.matmul(pb1[:], w1b[:, k, j * 128:(j + 1) * 128], h2T[:, k, g * 512:(g + 1) * 512], start=(k == 0), stop=(k == 7)) for k in range(8)],
                         reads=[w1k, "h2T%d" % g], writes=[pk1])
                    P.pe(lambda e, pb3=pb3, w3b=w3b, j=j, g=g: [e.matmul(pb3[:], w3b[:, k, j * 128:(j + 1) * 128], h2T[:, k, g * 512:(g + 1) * 512], start=(k == 0), stop=(k == 7)) for k in range(8)],
                         reads=[w3k, "h2T%d" % g], writes=[pk3])
                    sk, sil = srr.next()
                    P.act(lambda e, sil=sil, pb1=pb1: e.activation(out=sil[:], in_=pb1[:], func=AF.Silu), reads=[pk1], writes=[sk])
                    P.dve(lambda e, actT=actT, sil=sil, pb3=pb3, j=j: e.tensor_tensor(out=actT[:, j, :], in0=pb3[:], in1=sil[:], op=ALU.mult), reads=[pk3, sk], writes=[atk])
                if pend is not None:
                    emit_down(pend)
                pend = (ex, atk, actT, w2k, w2b, g)
    emit_down(pend)

    x1r = rr("x1r", [128, D], F32)
    y2r = rr("y2", [128, D], F32)
    st2 = rr("bst2", [128, 2, 6], F32)
    mv2 = rr("mv2", [128, 2], F32)
    rd2 = rr("rstd2", [128, 1], F32)
    for i in range(NT):
        r0 = i * 128
        xk, x1t = x1r.next()
        P.dma(xk, lambda e, x1t=x1t, r0=r0: e.dma_start(out=x1t[:], in_=x1_d[r0:r0 + 128, :]), reads=["x1d%d" % i], writes=[xk])
        yk, y2 = y2r.next()
        P.pool(lambda e, y2=y2, i=i: e.tensor_tensor(out=y2[:], in0=facc[:, i, :], in1=OPG2, op=ALU.mult), reads=["facc%d" % i, "modg2"], writes=[yk])
        P.dve(lambda e, y2=y2, x1t=x1t: e.scalar_tensor_tensor(out=y2[:], in0=x1t[:], scalar=ALPHA, in1=y2[:], op0=ALU.mult, op1=ALU.add), reads=[xk, yk], writes=[yk])
        stk, bst = st2.next()
        mvk, mv = mv2.next()
        rdk, rstd = rd2.next()
        P.dve(lambda e, bst=bst, y2=y2: [e.bn_stats(out=bst[:, 0, :], in_=y2[:, 0:512]), e.bn_stats(out=bst[:, 1, :], in_=y2[:, 512:1024])], reads=[yk], writes=[stk])
        P.dve(lambda e, mv=mv, bst=bst: e.bn_aggr(out=mv[:], in_=bst[:]), reads=[stk], writes=[mvk])
        P.act(lambda e, rstd=rstd, mv=mv: e.activation(out=rstd[:], in_=mv[:, 1:2], func=AF.Sqrt, bias=epst[:], scale=1.0), reads=[mvk, "epst"], writes=[rdk])
        P.dve(lambda e, rstd=rstd: e.reciprocal(out=rstd[:], in_=rstd[:]), reads=[rdk], writes=[rdk])
        P.dve(lambda e, y2=y2, mv=mv, rstd=rstd: e.tensor_scalar(out=y2[:], in0=y2[:], scalar1=mv[:, 0:1], scalar2=rstd[:, 0:1], op0=ALU.subtract, op1=ALU.mult), reads=[yk, mvk, rdk], writes=[yk])
        P.pool(lambda e, y2=y2: e.tensor_tensor(out=y2[:], in0=y2[:], in1=lnb2[:, 0, :], op=ALU.mult), reads=[yk, "lnb2"], writes=[yk])
        P.pool(lambda e, y2=y2: e.tensor_tensor(out=y2[:], in0=y2[:], in1=lnb2[:, 1, :], op=ALU.add), reads=[yk, "lnb2"], writes=[yk])
        P.dma(yk, lambda e, y2=y2, r0=r0: e.dma_start(out=xo_d[r0:r0 + 128, :], in_=y2[:]), reads=[yk], writes=["xo%d" % i])
        outs.append("xo%d" % i)
    P.op("sp", None, reads=outs)
    return P.emit()


S_FULL = 16384
NCORE = 8
TPC = S_FULL // NCORE
_PROGS = {}
_DBG = None


def _prog(key, fn):
    if key not in _PROGS:
        _PROGS[key] = fn()
    return _PROGS[key]


def _run(nc, in_maps):
    res = run_bass_kernel_spmd(nc, in_maps, core_ids=list(range(NCORE)))
    return res.results


def kernel(x, c, w_ada, b_ada, w_in, lam_qk, attn_norm_g, hg_lb_logits, hg_norm_g, pool_w, pool_scale,
           w_out, ln1_g, ln1_b, ln2_g, ln2_b, ffn_w1, ffn_w3, ffn_w2, router_w, exp_w1, exp_w3, exp_w2):
    f32 = np.float32
    A = lambda a: np.ascontiguousarray(np.asarray(a))
    x = A(x).astype(f32, copy=False)
    xs = x[0]
    ct = A(np.asarray(c, f32).reshape(8, 128).T)
    lbl = A(np.asarray(hg_lb_logits, f32))
    tric = hgrn_consts()
    mt, mtp = pool_mats(False)
    mt_first, _ = pool_mats(True)
    S_ = xs.shape[0]
    T = S_ // NCORE
    for l in range(2):
        wa = np.asarray(w_ada[l], f32)
        ba = np.asarray(b_ada[l], f32)
        nc1 = _prog(("p1", l, T), lambda: build_p1(T, l))
        winp = win_perm(np.asarray(w_in[l], f32))
        wada1 = A(wa[:, 0:2048]); bada1 = A(ba[0:2048])
        maps = []
        for ci in range(NCORE):
            cosT, sinT = rope_tables(np.arange(ci * T, (ci + 1) * T))
            maps.append({"x": A(xs[ci * T:(ci + 1) * T]), "ct": ct, "wada": wada1, "bada": bada1, "win": winp,
                         "cosT": cosT, "sinT": sinT, "lbl": lbl})
        r1 = _run(nc1, maps)
        QT = np.concatenate([r["QT"] for r in r1], 1)
        KT = np.concatenate([r["KT"] for r in r1], 1)
        V = np.concatenate([r["V"] for r in r1], 0)
        HQT = np.concatenate([r["HQT"] for r in r1], 1)
        SGT = np.concatenate([r["SGT"] for r in r1], 1)
        LOGF = np.concatenate([r["LOGF"] for r in r1], 0)
        VAL = np.concatenate([r["VAL"] for r in r1], 0)
        SG = np.concatenate([r["SG"] for r in r1], 0)
        U = np.concatenate([r["U"] for r in r1], 0)
        nc2 = _prog(("p2", l, S_), lambda: build_p2(S_, True, True, l))
        maps = []
        for n in range(NCORE):
            h, z = n // 2, n % 2
            maps.append({"QTn": A(QT[n * 64:(n + 1) * 64]), "KTn": A(KT[n * 64:(n + 1) * 64]), "Vh": A(V[:, h * 128:(h + 1) * 128]),
                         "HQTh": A(HQT[h * 64:(h + 1) * 64]), "SGTh": A(SGT[h * 64:(h + 1) * 64]), "LOGFh": A(LOGF[:, h * 64:(h + 1) * 64]),
                         "VALhz": A(VAL[:, h * 64 + z * 32:h * 64 + z * 32 + 32]), "lblh": A(lbl[:, h * 64:(h + 1) * 64].T), "TRIC": tric})
        r2 = _run(nc2, maps)
        OATT = np.concatenate([r["OATT"] for r in r2], 1)
        OHG = np.concatenate([r["OHG"] for r in r2], 1)
        if _DBG is not None:
            _DBG[l] = dict(QT=QT, KT=KT, V=V, HQT=HQT, SGT=SGT, LOGF=LOGF, VAL=VAL, SG=SG, U=U, OATT=OATT, OHG=OHG)
        moe = (l % 2 == 1)
        if moe:
            E, F, NJ = 8, 3584, 4
            w1 = np.asarray(exp_w1[l // 2], f32); w3 = np.asarray(exp_w3[l // 2], f32); w2 = np.asarray(exp_w2[l // 2], f32)
        else:
            E, F, NJ = 1, 2816, 2
            w1 = np.asarray(ffn_w1[l // 2], f32)[None]; w3 = np.asarray(ffn_w3[l // 2], f32)[None]; w2 = np.asarray(ffn_w2[l // 2], f32)[None]
        nc3 = _prog(("p3", l, T), lambda: build_p3(T, l, F, E, NJ))
        UH = np.concatenate([np.zeros((128, 256), f32), U], 0)
        base = {"ct": ct, "wada": A(wa[:, 2048:]), "bada": A(ba[2048:]), "lamqk": A(np.asarray(lam_qk[l], f32).reshape(-1)),
                "attng": A(np.asarray(attn_norm_g[l], f32)), "hgg": A(np.asarray(hg_norm_g[l], f32)),
                "poolw": A(np.asarray(pool_w[l], f32).transpose(1, 0, 2)), "pscale": A(np.asarray(pool_scale[l], f32)),
                "wout": A(np.asarray(w_out[l], f32)),
                "lnp": A(np.stack([np.asarray(ln1_g[l], f32), np.asarray(ln1_b[l], f32), np.asarray(ln2_g[l], f32), np.asarray(ln2_b[l], f32)])),
                "MT": mt, "MTP": mtp, "w1": A(w1), "w3": A(w3), "w2": A(w2)}
        if moe:
            base["rw"] = A(np.asarray(router_w[l // 2], f32).reshape(8, 128, 8).transpose(1, 0, 2))
        maps = []
        for ci in range(NCORE):
            m = dict(base)
            sl = slice(ci * T, (ci + 1) * T)
            m.update({"x": A(xs[sl]), "OATT": A(OATT[sl]), "OHG": A(OHG[sl]), "SG": A(SG[sl]), "UH": A(UH[ci * T:(ci + 1) * T + 128]),
                      "MT0": mt_first if ci == 0 else mt})
            maps.append(m)
        r3 = _run(nc3, maps)
        xs = np.concatenate([r["xo"] for r in r3], 0)
        if _DBG is not None:
            _DBG[l]["xo"] = xs
    return xs[None].astype(f32, copy=False)
```

```python
import numpy as np
from contextlib import ExitStack
import concourse.bass as bass
import concourse.mybir as mybir
from concourse.bass_utils import run_bass_kernel_spmd

F32 = mybir.dt.float32
BF16 = mybir.dt.bfloat16
AF = mybir.ActivationFunctionType
ALU = mybir.AluOpType
AX = mybir.AxisListType


class _Op:
    __slots__ = ("eng", "fn", "deps", "dma", "ndma", "signal", "sem", "val", "idx", "inc")


class Prog:
    ENGS = ("pe", "act", "dve", "pool", "sp")

    def __init__(self):
        self.nc = bass.Bass("TRN2", target_bir_lowering=False)
        self.es = ExitStack()
        self.ops = []
        self.last_w = {}
        self.readers = {}
        self.bar = None
        self.oldr = {}
        self.pw_prefixes = ()
        self.group_alias = {}
        self.capture = None
        self.scopes = []
        self._uid = 0
        self._bar_t = self.es.enter_context(self.nc.sbuf_tensor("sb__bar", [1, 8], F32))

    def sb(self, name, shape, dt):
        es = self.scopes[-1] if self.scopes else self.es
        self._uid += 1
        return es.enter_context(self.nc.sbuf_tensor("sb_%s_%d" % (name, self._uid), list(shape), dt))

    def open_scope(self):
        self.scopes.append(ExitStack())

    def close_scope(self):
        self.scopes.pop().close()
        self.barrier()

    def barrier(self):
        keys = list(set(list(self.last_w.keys()) + list(self.readers.keys())))
        t = self._bar_t
        o = self.op("dve", lambda e: e.memset(t[:], 0.0), reads=keys, writes=["__bar"])
        self.bar = o.idx

    def ps(self, name, shape, dt):
        return self.es.enter_context(self.nc.psum_tensor("ps_" + name, list(shape), dt))

    def dram(self, name, shape, dt, kind="Internal"):
        return self.nc.dram_tensor(name, list(shape), dt, kind=kind).ap()

    def op(self, eng, fn, reads=(), writes=(), dma=None, ndma=1, inc=16, pw=()):
        if self.capture is not None:
            self.capture.append((eng, fn, tuple(reads), tuple(writes), dma, ndma, inc, tuple(pw)))
            return None
        o = _Op()
        o.inc = inc
        o.eng = eng
        o.fn = fn
        o.dma = self.group_alias.get(dma, dma) if dma is not None else None
        o.ndma = ndma if dma is not None else 0
        o.signal = False
        o.sem = None
        o.val = 0
        o.idx = len(self.ops)
        if self.pw_prefixes:
            pw = set(pw) | set(k for k in writes if k.startswith(self.pw_prefixes))
        deps = set()
        for k in reads:
            deps.update(self.last_w.get(k, ()))
        for k in writes:
            deps.update(self.readers.get(k, ()))
            if k not in pw:
                deps.update(self.last_w.get(k, ()))
            else:
                deps.update(self.oldr.get(k, ()))
        if self.bar is not None:
            deps.add(self.bar)
        deps.discard(o.idx)
        o.deps = deps
        for k in reads:
            self.readers.setdefault(k, []).append(o.idx)
        for k in writes:
            if k in pw and not self.readers.get(k):
                self.last_w.setdefault(k, []).append(o.idx)
            else:
                self.oldr[k] = list(self.readers.get(k, ())) if k in pw else []
                self.last_w[k] = [o.idx]
            self.readers[k] = []
        self.ops.append(o)
        return o

    def pe(self, fn, reads=(), writes=()):
        return self.op("pe", fn, reads, writes)

    def act(self, fn, reads=(), writes=()):
        return self.op("act", fn, reads, writes)

    def dve(self, fn, reads=(), writes=()):
        return self.op("dve", fn, reads, writes)

    def pool(self, fn, reads=(), writes=()):
        return self.op("pool", fn, reads, writes)

    def dma(self, group, fn, reads=(), writes=(), eng="sp", ndma=1, pw=()):
        return self.op(eng, fn, reads, writes, dma=group, ndma=ndma, pw=pw)

    def replay(self, items):
        for it in items:
            self.op(*it)

    def cc(self, group, fn, reads=(), writes=()):
        return self.op("pool", fn, reads, writes, dma=group, ndma=1, inc=1)

    def emit(self):
        nc = self.nc
        ops = self.ops

        def needs(d, o):
            if d.dma is not None:
                return True
            if d.eng == "pe" and o.eng == "pe" and o.dma is None:
                return False
            return True

        for o in ops:
            for di in o.deps:
                d = ops[di]
                if needs(d, o):
                    d.signal = True
        cnt = {}
        for o in ops:
            if o.dma is not None:
                key = "d:" + o.dma
                cnt[key] = cnt.get(key, 0) + o.inc * o.ndma
                o.sem = key
                o.val = cnt[key]
            elif o.signal:
                key = "e:" + o.eng
                cnt[key] = cnt.get(key, 0) + 1
                o.sem = key
                o.val = cnt[key]
        import bisect
        ghist = {}
        for o in ops:
            if o.dma is not None:
                ghist.setdefault(o.sem, ([], []))
                ghist[o.sem][0].append(o.idx)
                ghist[o.sem][1].append(o.val)

        def dma_wait_val(d, o):
            idxs, vals = ghist[d.sem]
            j = bisect.bisect_left(idxs, o.idx) - 1
            return vals[j]
        sems = {}
        for key in cnt:
            sems[key] = self.es.enter_context(nc.semaphore("s_" + key.replace(":", "_")))
        self.nsem = len(sems)
        block = self.es.enter_context(nc.Block())
        by_eng = {e: [o for o in ops if o.eng == e] for e in self.ENGS}

        def run_stream(engname, e):
            waited = {}
            for o in by_eng[engname]:
                need = {}
                for di in o.deps:
                    d = ops[di]
                    if not needs(d, o):
                        continue
                    v = dma_wait_val(d, o) if d.dma is not None else d.val
                    if need.get(d.sem, 0) < v:
                        need[d.sem] = v
                for s, v in need.items():
                    if waited.get(s, 0) < v:
                        e.wait_ge(sems[s], v)
                        waited[s] = v
                if o.fn is None:
                    continue
                r = o.fn(e)
                if o.dma is not None:
                    ins = r if isinstance(r, (list, tuple)) else [r]
                    assert len(ins) == o.ndma, (len(ins), o.ndma)
                    for i in ins:
                        i.then_inc(sems[o.sem], o.inc)
                elif o.signal:
                    i = r[-1] if isinstance(r, (list, tuple)) else r
                    i.then_inc(sems[o.sem], 1)

        if by_eng["pe"]:
            @block.tensor
            def _(e):
                run_stream("pe", e)
        if by_eng["act"]:
            @block.scalar
            def _(e):
                run_stream("act", e)
        if by_eng["dve"]:
            @block.vector
            def _(e):
                run_stream("dve", e)
        if by_eng["pool"]:
            @block.gpsimd
            def _(e):
                run_stream("pool", e)
        if by_eng["sp"]:
            @block.sync
            def _(e):
                run_stream("sp", e)
        self.es.close()
        return nc

import math

D=1024
def rope_tables(pos):
    inv = (1.0 / (np.float32(10000.0) ** (np.arange(0, 64, 2, dtype=np.float32) / np.float32(64)))).astype(np.float32)
    ang = pos.astype(np.float32)[:, None] * inv[None, :]
    c = np.cos(ang.astype(np.float64)).astype(np.float32)
    s = np.sin(ang.astype(np.float64)).astype(np.float32)
    cos64 = np.concatenate([c, c], 1)
    sin64 = np.concatenate([-s, s], 1)
    cosT = np.ascontiguousarray(np.concatenate([cos64, cos64], 1).T)
    sinT = np.ascontiguousarray(np.concatenate([sin64, sin64], 1).T)
    return cosT, sinT

def win_perm(w_in_l):
    q, k, v, hq, hf, hi, hg, u = np.split(w_in_l, [512, 1024, 1536, 1792, 2048, 2304, 2560], axis=1)
    def sw(w):
        w4 = w.reshape(1024, 8, 2, 32)
        return w4[:, :, ::-1, :].reshape(1024, 512)
    return np.ascontiguousarray(np.concatenate([q, k, v, hq, hf, u, hi, hg, sw(q), sw(k)], 1))

def hgrn_consts():
    s = np.arange(64)[:, None]; t = np.arange(64)[None, :]
    tric = (s <= t).astype(np.float32) - (s <= 31).astype(np.float32)
    ext = np.stack([(np.arange(64) <= 31), np.ones(64), (np.arange(64) > 31), np.zeros(64)], 1).astype(np.float32)
    return np.ascontiguousarray(np.concatenate([tric, ext], 1))

POOL_WINDOWS = (2, 4, 8, 16)
def pool_mats(first):
    cur = np.zeros((128, 4, 128), np.float32); prev = np.zeros((128, 4, 128), np.float32)
    for g, win in enumerate(POOL_WINDOWS):
        for t in range(128):
            cnt = min(win, t + 1) if first else win
            for d in range(cnt):
                s = t - d
                if s >= 0:
                    cur[s, g, t] += 1.0 / cnt
                else:
                    prev[128 + s, g, t] += 1.0 / cnt
            cur[t, g, t] -= 1.0
    return cur, prev


D = 1024
NW = 3840
C_Q, C_K, C_V, C_HQ, C_HF, C_U, C_HI, C_HG, C_QS, C_KS = 0, 512, 1024, 1536, 1792, 2048, 2304, 2560, 2816, 3328


class RR:
    def __init__(self, items):
        self.items = items
        self.i = 0

    def next(self):
        it = self.items[self.i % len(self.items)]
        self.i += 1
        return it


def make_ident(P, dt=BF16):
    identf = P.sb("identf", [128, 128], F32)
    ident = P.sb("ident", [128, 128], dt)

    P.pool(lambda e: e.memset(identf[:], 1.0), writes=["identf"])
    P.pool(lambda e: e.affine_select(out=identf[:], in_=identf[:], pattern=[[-1, 128]], compare_op=ALU.is_equal,
                                     fill=0.0, base=0, channel_multiplier=1), reads=["identf"], writes=["identf"])
    P.dve(lambda e: e.tensor_copy(out=ident[:], in_=identf[:]), reads=["identf"], writes=["ident"])
    return ident, identf


def build_p1(T, layer):
    P = Prog()
    G = T // 512
    x_d = P.dram("x", [T, D], F32, "ExternalInput")
    ct_d = P.dram("ct", [128, 8], F32, "ExternalInput")
    wada_d = P.dram("wada", [D, 2048], F32, "ExternalInput")
    bada_d = P.dram("bada", [2048], F32, "ExternalInput")
    win_d = P.dram("win", [D, NW], F32, "ExternalInput")
    cos_d = P.dram("cosT", [128, T], F32, "ExternalInput")
    sin_d = P.dram("sinT", [128, T], F32, "ExternalInput")
    lbl_d = P.dram("lbl", [2, 256], F32, "ExternalInput")
    QT_d = P.dram("QT", [512, T], BF16, "ExternalOutput")
    KT_d = P.dram("KT", [512, T], BF16, "ExternalOutput")
    V_d = P.dram("V", [T, 512], BF16, "ExternalOutput")
    HQT_d = P.dram("HQT", [256, T], F32, "ExternalOutput")
    SGT_d = P.dram("SGT", [256, T], F32, "ExternalOutput")
    LOGF_d = P.dram("LOGF", [T, 256], F32, "ExternalOutput")
    VAL_d = P.dram("VAL", [T, 256], BF16, "ExternalOutput")
    SG_d = P.dram("SG", [T, 256], F32, "ExternalOutput")
    U_d = P.dram("U", [T, 256], F32, "ExternalOutput")
    outs = []

    ident, _ = make_ident(P)
    ones = P.sb("ones", [128, 1], F32)
    P.pool(lambda e: e.memset(ones[:], 1.0), writes=["ones"])

    wbf = P.sb("wbf", [128, 8, NW], BF16)
    for j in range(8):
        P.dma("wbf%d" % j, lambda e, j=j: e.dma_start(out=wbf[:, j, :], in_=win_d[j * 128:(j + 1) * 128, :]),
              writes=["wbf%d" % j], eng="pool")
    wkeys = ["wbf%d" % j for j in range(8)]

    ct = P.sb("ct", [128, 8], F32)
    cond = P.sb("cond", [128, 8], F32)
    condb = P.sb("condb", [128, 8, 128], F32)
    P.dma("ct", lambda e: e.dma_start(out=ct[:], in_=ct_d), writes=["ct"])
    P.act(lambda e: e.activation(out=cond[:], in_=ct[:], func=AF.Silu), reads=["ct"], writes=["cond"])
    P.dve(lambda e: e.tensor_copy(out=condb[:], in_=cond[:].unsqueeze(2).to_broadcast([128, 8, 128])),
          reads=["cond"], writes=["condb"])
    mod = P.sb("mod", [128, 2048], F32)
    badab = P.sb("badab", [128, 2048], F32)
    P.dma("badab", lambda e: e.dma_start(out=badab[:], in_=bada_d.partition_broadcast(128)), writes=["badab"])
    wst = [("wst%d" % i, P.sb("wst%d" % i, [128, 512], F32)) for i in range(3)]
    wrr = RR(wst)
    pbs = [("pb%d" % i, P.ps("pb%d" % i, [128, 512], F32)) for i in range(7)]
    prr = RR(pbs)
    psT = P.ps("psT", [128, 1024], BF16)
    for n in range(4):
        pk, pb = prr.next()
        for k in range(8):
            wk, wt = wrr.next()
            P.dma(wk, lambda e, wt=wt, k=k, n=n: e.dma_start(out=wt[:], in_=wada_d[k * 128:(k + 1) * 128, n * 512:(n + 1) * 512]),
                  writes=[wk])
            P.pe(lambda e, pb=pb, wt=wt, k=k: e.matmul(pb[:], condb[:, k, :], wt[:], start=(k == 0), stop=(k == 7)),
                 reads=[wk, "condb"], writes=[pk])
        P.dve(lambda e, pb=pb, n=n: e.tensor_tensor(out=mod[:, n * 512:(n + 1) * 512], in0=pb[:], in1=badab[:, n * 512:(n + 1) * 512], op=ALU.add),
              reads=[pk, "badab"], writes=["mod"])
    P.dve(lambda e: e.tensor_scalar_add(out=mod[:, 1024:2048], in0=mod[:, 1024:2048], scalar1=1.0), reads=["mod"], writes=["mod"])

    cosT = P.sb("cosT_s", [128, T], F32)
    sinT = P.sb("sinT_s", [128, T], F32)
    P.dma("cosT", lambda e: e.dma_start(out=cosT[:], in_=cos_d), writes=["cosT"])
    P.dma("sinT", lambda e: e.dma_start(out=sinT[:], in_=sin_d), writes=["sinT"])

    if layer == 1:
        lbb = P.sb("lbb", [128, 2, 256], F32)
        lb = P.sb("lb", [128, 256], F32)
        P.dma("lbb", lambda e: e.dma_start(out=lbb[:], in_=lbl_d.rearrange("a b -> (a b)").partition_broadcast(128)), writes=["lbb"])
        P.dve(lambda e: e.tensor_tensor(out=lb[:], in0=lbb[:, 1, :], in1=lbb[:, 0, :], op=ALU.subtract), reads=["lbb"], writes=["lb"])
        P.act(lambda e: e.activation(out=lb[:], in_=lb[:], func=AF.Sigmoid), reads=["lb"], writes=["lb"])

    xrr = RR([("xt%d" % i, P.sb("xt%d" % i, [128, D], F32)) for i in range(3)])
    hrr = RR([("hb%d" % i, P.sb("hb%d" % i, [128, D], BF16)) for i in range(2)])
    htrr = RR([("hT%d" % i, P.sb("hT%d" % i, [128, 8, 512], BF16)) for i in range(2)])
    tmprr = RR([("tm%d" % i, P.sb("tm%d" % i, [128, D], F32)) for i in range(2)])
    vrr = RR([("vt%d" % i, P.sb("vt%d" % i, [128, 512], BF16)) for i in range(2)])
    err_ = RR([("et%d" % i, P.sb("et%d" % i, [128, 256], F32)) for i in range(2)])
    l1rr = RR([("l1%d" % i, P.sb("l1%d" % i, [128, 256], F32)) for i in range(2)])
    l2rr = RR([("l2%d" % i, P.sb("l2%d" % i, [128, 256], F32)) for i in range(2)])
    lfrr = RR([("lf%d" % i, P.sb("lf%d" % i, [128, 256], F32)) for i in range(2)])
    valrr = RR([("va%d" % i, P.sb("va%d" % i, [128, 256], BF16)) for i in range(2)])
    sgrr = RR([("sg%d" % i, P.sb("sg%d" % i, [128, 256], F32)) for i in range(2)])
    urr = RR([("ut%d" % i, P.sb("ut%d" % i, [128, 256], F32)) for i in range(2)])
    r1rr = RR([("r1%d" % i, P.sb("r1%d" % i, [128, 512], F32)) for i in range(2)])
    r2rr = RR([("r2%d" % i, P.sb("r2%d" % i, [128, 512], F32)) for i in range(2)])
    qrrr = RR([("qr%d" % i, P.sb("qr%d" % i, [128, 512], BF16)) for i in range(3)])
    fmrr = RR([("fm%d" % i, P.sb("fm%d" % i, [128, 512], F32)) for i in range(3)])

    for g in range(G):
        hk, hT = htrr.next()
        for tt in range(4):
            r0 = g * 512 + tt * 128
            xk, xt = xrr.next()
            P.dma(xk, lambda e, xt=xt, r0=r0: e.dma_start(out=xt[:], in_=x_d[r0:r0 + 128, :]), writes=[xk])
            tk, tm = tmprr.next()
            bk, hb = hrr.next()
            P.dve(lambda e, tm=tm, xt=xt: e.tensor_tensor(out=tm[:], in0=xt[:], in1=mod[:, 1024:2048], op=ALU.mult),
                  reads=[xk, "mod"], writes=[tk])
            P.pool(lambda e, tm=tm, hb=hb: e.tensor_tensor(out=hb[:], in0=tm[:], in1=mod[:, 0:1024], op=ALU.add),
                   reads=[tk, "mod"], writes=[bk])
            P.pe(lambda e, hb=hb: [e.transpose(psT[:, k * 128:(k + 1) * 128], hb[:, k * 128:(k + 1) * 128], ident[:]) for k in range(8)],
                 reads=[bk, "ident"], writes=["psT"])
            P.act(lambda e, hT=hT, tt=tt: e.copy(out=hT[:, :, tt * 128:(tt + 1) * 128], in_=psT[:].rearrange("p (k t) -> p k t", k=8)),
                  reads=["psT"], writes=[hk])
            pkv, pbv = prr.next()
            P.pe(lambda e, pbv=pbv, hT=hT, tt=tt: [e.matmul(pbv[:], hT[:, k, tt * 128:(tt + 1) * 128], wbf[:, k, C_V:C_V + 512], start=(k == 0), stop=(k == 7)) for k in range(8)],
                 reads=[hk] + wkeys, writes=[pkv])
            vk, vt = vrr.next()
            P.act(lambda e, vt=vt, pbv=pbv: e.copy(out=vt[:], in_=pbv[:]), reads=[pkv], writes=[vk])
            P.dma(vk, lambda e, vt=vt, r0=r0: e.dma_start(out=V_d[r0:r0 + 128, :], in_=vt[:]), reads=[vk], writes=["oV%d" % r0])
            outs.append("oV%d" % r0)
            pkb, pbb = prr.next()
            P.pe(lambda e, pbb=pbb, hT=hT, tt=tt: [e.matmul(pbb[:], hT[:, k, tt * 128:(tt + 1) * 128], wbf[:, k, C_HF:C_HF + 512], start=(k == 0), stop=(k == 7)) for k in range(8)],
                 reads=[hk] + wkeys, writes=[pkb])
            ek, et = err_.next()
            P.act(lambda e, et=et, pbb=pbb: e.activation(out=et[:], in_=pbb[:, 0:256], func=AF.Exp, scale=-1.0), reads=[pkb], writes=[ek])
            uk, ut = urr.next()
            P.dve(lambda e, ut=ut, pbb=pbb: e.tensor_copy(out=ut[:], in_=pbb[:, 256:512]), reads=[pkb], writes=[uk])
            P.dma(uk, lambda e, ut=ut, r0=r0: e.dma_start(out=U_d[r0:r0 + 128, :], in_=ut[:]), reads=[uk], writes=["oU%d" % r0])
            outs.append("oU%d" % r0)
            l1k, l1 = l1rr.next()
            P.act(lambda e, l1=l1, et=et: e.activation(out=l1[:], in_=et[:], func=AF.Ln, bias=ones[:], scale=1.0), reads=[ek, "ones"], writes=[l1k])
            lfk, lf = lfrr.next()
            if layer == 0:
                P.pool(lambda e, lf=lf, l1=l1: e.tensor_scalar(out=lf[:], in0=l1[:], scalar1=-1.0, scalar2=None, op0=ALU.mult), reads=[l1k], writes=[lfk])
            else:
                l2k, l2 = l2rr.next()
                P.pool(lambda e, l2=l2, et=et: e.tensor_tensor(out=l2[:], in0=et[:], in1=lb[:], op=ALU.mult), reads=[ek, "lb"], writes=[l2k])
                P.act(lambda e, l2=l2: e.activation(out=l2[:], in_=l2[:], func=AF.Ln, bias=ones[:], scale=1.0), reads=[l2k, "ones"], writes=[l2k])
                P.pool(lambda e, lf=lf, l1=l1, l2=l2: e.tensor_tensor(out=lf[:], in0=l2[:], in1=l1[:], op=ALU.subtract), reads=[l1k, l2k], writes=[lfk])
            P.dma(lfk, lambda e, lf=lf, r0=r0: e.dma_start(out=LOGF_d[r0:r0 + 128, :], in_=lf[:]), reads=[lfk], writes=["oLF%d" % r0])
            outs.append("oLF%d" % r0)
            pkc, pbc = prr.next()
            P.pe(lambda e, pbc=pbc, hT=hT, tt=tt: [e.matmul(pbc[:], hT[:, k, tt * 128:(tt + 1) * 128], wbf[:, k, C_HI:C_HI + 512], start=(k == 0), stop=(k == 7)) for k in range(8)],
                 reads=[hk] + wkeys, writes=[pkc])
            vak, va = valrr.next()
            sgk, sg = sgrr.next()
            P.act(lambda e, va=va, pbc=pbc: e.activation(out=va[:], in_=pbc[:, 0:256], func=AF.Silu), reads=[pkc], writes=[vak])
            P.act(lambda e, sg=sg, pbc=pbc: e.activation(out=sg[:], in_=pbc[:, 256:512], func=AF.Silu), reads=[pkc], writes=[sgk])
            P.dma(vak, lambda e, va=va, r0=r0: e.dma_start(out=VAL_d[r0:r0 + 128, :], in_=va[:]), reads=[vak], writes=["oVA%d" % r0])
            P.dma(sgk, lambda e, sg=sg, r0=r0: e.dma_start(out=SG_d[r0:r0 + 128, :], in_=sg[:]), reads=[sgk], writes=["oSG%d" % r0])
            outs += ["oVA%d" % r0, "oSG%d" % r0]
        c0 = g * 512

        def fm_mm(pb, col, hT=hT):
            return lambda e: [e.matmul(pb[:], wbf[:, k, col:col + 128], hT[:, k, :], start=(k == 0), stop=(k == 7)) for k in range(8)]
        for which, cbase, csw, out_d in (("q", C_Q, C_QS, QT_d), ("k", C_K, C_KS, KT_d)):
            for mp in range(4):
                pk1, pb1 = prr.next()
                pk2, pb2 = prr.next()
                P.pe(fm_mm(pb1, cbase + mp * 128), reads=[hk] + wkeys, writes=[pk1])
                P.pe(fm_mm(pb2, csw + mp * 128), reads=[hk] + wkeys, writes=[pk2])
                r1k, r1 = r1rr.next()
                r2k, r2 = r2rr.next()
                P.dve(lambda e, r1=r1, pb1=pb1, c0=c0: e.tensor_tensor(out=r1[:], in0=pb1[:], in1=cosT[:, c0:c0 + 512], op=ALU.mult), reads=[pk1, "cosT"], writes=[r1k])
                P.dve(lambda e, r2=r2, pb2=pb2, c0=c0: e.tensor_tensor(out=r2[:], in0=pb2[:], in1=sinT[:, c0:c0 + 512], op=ALU.mult), reads=[pk2, "sinT"], writes=[r2k])
                qk_, qr = qrrr.next()
                P.pool(lambda e, qr=qr, r1=r1, r2=r2: e.tensor_tensor(out=qr[:], in0=r1[:], in1=r2[:], op=ALU.add), reads=[r1k, r2k], writes=[qk_])
                ok = "o%s%d_%d" % (which, g, mp)
                P.dma(qk_, lambda e, qr=qr, out_d=out_d, mp=mp, c0=c0: e.dma_start(out=out_d[mp * 128:(mp + 1) * 128, c0:c0 + 512], in_=qr[:]), reads=[qk_], writes=[ok])
                outs.append(ok)
        for hc in range(2):
            pk1, pb1 = prr.next()
            P.pe(fm_mm(pb1, C_HQ + hc * 128), reads=[hk] + wkeys, writes=[pk1])
            fk, fm = fmrr.next()
            P.dve(lambda e, fm=fm, pb1=pb1: e.tensor_copy(out=fm[:], in_=pb1[:]), reads=[pk1], writes=[fk])
            ok = "ohq%d_%d" % (g, hc)
            P.dma(fk, lambda e, fm=fm, hc=hc, c0=c0: e.dma_start(out=HQT_d[hc * 128:(hc + 1) * 128, c0:c0 + 512], in_=fm[:]), reads=[fk], writes=[ok])
            outs.append(ok)
            pk2, pb2 = prr.next()
            P.pe(fm_mm(pb2, C_HF + hc * 128), reads=[hk] + wkeys, writes=[pk2])
            fk, fm = fmrr.next()
            P.act(lambda e, fm=fm, pb2=pb2: e.activation(out=fm[:], in_=pb2[:], func=AF.Sigmoid, scale=-1.0), reads=[pk2], writes=[fk])
            ok = "osg%d_%d" % (g, hc)
            P.dma(fk, lambda e, fm=fm, hc=hc, c0=c0: e.dma_start(out=SGT_d[hc * 128:(hc + 1) * 128, c0:c0 + 512], in_=fm[:]), reads=[fk], writes=[ok])
            outs.append(ok)
    P.op("sp", None, reads=outs)
    return P.emit()


def build_p2(S, do_attn=True, do_hgrn=True, layer=0):
    P = Prog()
    NB = S // 128
    NQ = S // 512
    QT_d = P.dram("QTn", [64, S], BF16, "ExternalInput")
    KT_d = P.dram("KTn", [64, S], BF16, "ExternalInput")
    V_d = P.dram("Vh", [S, 128], BF16, "ExternalInput")
    OA_d = P.dram("OATT", [128, S], F32, "ExternalOutput")
    HQ_d = P.dram("HQTh", [64, S], F32, "ExternalInput")
    SG_d = P.dram("SGTh", [64, S], F32, "ExternalInput")
    LF_d = P.dram("LOGFh", [S, 64], F32, "ExternalInput")
    VA_d = P.dram("VALhz", [S, 32], BF16, "ExternalInput")
    LB_d = P.dram("lblh", [64, 2], F32, "ExternalInput")
    TR_d = P.dram("TRIC", [64, 68], F32, "ExternalInput")
    OH_d = P.dram("OHG", [S, 32], F32, "ExternalOutput")
    outs = []

    stp = P.ps("stp", [128, 1024], F32)
    oacc = [("oa0", P.ps("oa0", [128, 2, 256], F32))]
    denp = P.ps("denp", [128, 512], F32)
    hb = [("hb%d" % i, P.ps("hb%d" % i, [128, 512], F32)) for i in range(3)]
    ptp = P.ps("ptp", [128, 1024], BF16)
    A_ops, H_ops = [], []
    ident, identf = make_ident(P)

    if do_attn:
        QT = P.sb("QT", [64, S], BF16)
        KT = P.sb("KT", [64, S], BF16)
        V1 = P.sb("V1", [128, NB, 129], BF16)
        nsp = max(1, S // 4096)
        for i in range(nsp):
            a, b = i * (S // nsp), (i + 1) * (S // nsp)
            P.dma("QT%d" % i, lambda e, a=a, b=b: e.dma_start(out=QT[:, a:b], in_=QT_d[:, a:b]), writes=["QT%d" % i])
            P.dma("KT%d" % i, lambda e, a=a, b=b: e.dma_start(out=KT[:, a:b], in_=KT_d[:, a:b]), writes=["KT%d" % i])
            ba, bb = a // 128, b // 128
            P.dma("V1%d" % i, lambda e, a=a, b=b, ba=ba, bb=bb: e.dma_start(out=V1[:, ba:bb, 0:128], in_=V_d[a:b, :].rearrange("(b p) d -> p b d", p=128)),
                  writes=["V1%d" % i])
            P.pool(lambda e, ba=ba, bb=bb: e.memset(V1[:, ba:bb, 128:129], 1.0), writes=["V1o%d" % i])

        def seg(tok):
            return min(tok // (S // nsp), nsp - 1)
        trif = P.sb("trif", [128, 128], F32)
        tri = P.sb("tri", [128, 128], BF16)

        P.pool(lambda e: e.memset(trif[:], 1.0), writes=["trif"])
        P.pool(lambda e: e.affine_select(out=trif[:], in_=trif[:], pattern=[[1, 128]], compare_op=ALU.is_ge, fill=0.0, base=0, channel_multiplier=-1),
               reads=["trif"], writes=["trif"])
        P.dve(lambda e: e.tensor_copy(out=tri[:], in_=trif[:]), reads=["trif"], writes=["tri"])
        ptrr = RR([("pt%d" % i, P.sb("pt%d" % i, [128, 1024], BF16)) for i in range(3)])
        osrr = RR([("os%d" % i, P.sb("os%d" % i, [128, 512], F32)) for i in range(2)])
        rbrr = RR([("rb%d" % i, P.sb("rb%d" % i, [128, 512], F32)) for i in range(2)])
        onesb = P.sb("onesb", [128, 128], BF16)
        P.pool(lambda e: e.memset(onesb[:], 1.0), writes=["onesb"])
        its = [(qt, kp) for qt in range(NQ) for kp in range(2 * qt + 2)]
        pend = []

        def emit_st(qt, kp):
            q0 = qt * 512
            kbs = (2 * kp, 2 * kp + 1)
            P.pe(lambda e: [e.matmul(stp[:, h * 512:(h + 1) * 512], KT[:, kbs[h] * 128:(kbs[h] + 1) * 128], QT[:, q0:q0 + 512], start=True, stop=True) for h in range(2)],
                 reads=["KT%d" % seg(kbs[0] * 128), "QT%d" % seg(q0)], writes=["stp"])
            pk, ptt = ptrr.next()
            P.act(lambda e: e.activation(out=ptt[:], in_=stp[:], func=AF.Exp, scale=0.125), reads=["stp"], writes=[pk])
            for h in range(2):
                j = kbs[h] - 4 * qt
                if j >= 0:
                    o0 = h * 512 + j * 128
                    P.pool(lambda e, o0=o0: e.tensor_tensor(out=ptt[:, o0:o0 + 128], in0=ptt[:, o0:o0 + 128], in1=tri[:], op=ALU.mult), reads=[pk, "tri"], writes=[pk])
            return (qt, kp, pk, ptt)

        def emit_pv(item):
            qt, kp, pk, ptt = item
            ok_, ot = oacc[0]
            otv = ot[:].rearrange("p a b -> p (a b)")
            for h in range(2):
                kb = 2 * kp + h
                j = kb - 4 * qt
                c0 = 0 if j < 0 else 128 * j
                P.pe(lambda e, kb=kb, h=h, c0=c0: [
                    e.matmul(otv[:, c0:512], V1[:, kb, 0:128], ptt[:, h * 512 + c0:(h + 1) * 512], start=(kb == 0), stop=(kb == 4 * qt + 3), skip_group_check=True),
                    e.matmul(denp[:, c0:512], onesb[:], ptt[:, h * 512 + c0:(h + 1) * 512], start=(kb == 0), stop=(kb == 4 * qt + 3), skip_group_check=True)],
                    reads=[pk, "V1%d" % seg(kb * 128), "onesb"], writes=[ok_, "denp"])
            if kp == 2 * qt + 1:
                rk, rb = rbrr.next()
                sk_, osb = osrr.next()
                P.dve(lambda e: e.reciprocal(out=rb[:], in_=denp[:]), reads=["denp"], writes=[rk])
                P.dve(lambda e: e.tensor_tensor(out=osb[:], in0=otv, in1=rb[:], op=ALU.mult), reads=[ok_, rk], writes=[sk_])
                P.dma(sk_, lambda e: e.dma_start(out=OA_d[:, qt * 512:(qt + 1) * 512], in_=osb[:]), reads=[sk_], writes=["oOA%d" % qt])
                outs.append("oOA%d" % qt)
        LOOK = 1
        P.capture = A_ops
        for i, (qt, kp) in enumerate(its):
            pend.append(emit_st(qt, kp))
            if len(pend) > LOOK:
                emit_pv(pend.pop(0))
        while pend:
            emit_pv(pend.pop(0))
        P.capture = None

    if do_hgrn:
        NBAT = S // 512
        tr = P.sb("tr", [64, 68], F32)
        P.dma("tr", lambda e: e.dma_start(out=tr[:], in_=TR_d), writes=["tr"])
        lbt = P.sb("lbt", [64, 2], F32)
        oml = P.sb("oml", [64, 1], F32)
        if layer == 0:
            P.pool(lambda e: e.memset(oml[:], 1.0), writes=["oml"])
        else:
            P.dma("lbt", lambda e: e.dma_start(out=lbt[:], in_=LB_d), writes=["lbt"])
            P.dve(lambda e: e.tensor_tensor(out=oml[:], in0=lbt[:, 0:1], in1=lbt[:, 1:2], op=ALU.subtract), reads=["lbt"], writes=["oml"])
            P.act(lambda e: e.activation(out=oml[:], in_=oml[:], func=AF.Sigmoid), reads=["oml"], writes=["oml"])
        mk8f = P.sb("mk8f", [64, 8, 64], F32)

        P.pool(lambda e: e.memset(mk8f[:], 1.0), writes=["mk8f"])
        P.pool(lambda e: e.affine_select(out=mk8f[:], in_=mk8f[:], pattern=[[0, 8], [1, 64]], compare_op=ALU.is_ge, fill=0.0, base=0, channel_multiplier=-1),
               reads=["mk8f"], writes=["mk8f"])
        state = P.sb("state", [64, 32], F32)
        P.pool(lambda e: e.memset(state[:], 0.0), writes=["state"])
        smrr = RR([("sm%d" % i, P.sb("sm%d" % i, [64, 32], BF16)) for i in range(3)])
        lfrr = RR([("hlf%d" % i, P.sb("hlf%d" % i, [64, 8, 64], F32)) for i in range(2)])
        hqrr = RR([("hhq%d" % i, P.sb("hhq%d" % i, [64, 512], F32)) for i in range(2)])
        sgrr = RR([("hsg%d" % i, P.sb("hsg%d" % i, [64, 512], F32)) for i in range(2)])
        varr = RR([("hva%d" % i, P.sb("hva%d" % i, [64, 8, 32], BF16)) for i in range(2)])
        e1rr = RR([("he1%d" % i, P.sb("he1%d" % i, [64, 512], F32)) for i in range(2)])
        e2rr = RR([("he2%d" % i, P.sb("he2%d" % i, [64, 512], F32)) for i in range(2)])
        exrr = RR([("hex%d" % i, P.sb("hex%d" % i, [64, 8, 4], F32)) for i in range(2)])
        qdrr = RR([("hqd%d" % i, P.sb("hqd%d" % i, [64, 512], BF16)) for i in range(2)])
        kdrr = RR([("hkd%d" % i, P.sb("hkd%d" % i, [64, 512], BF16)) for i in range(2)])
        ktrr = RR([("hkt%d" % i, P.sb("hkt%d" % i, [64, 512], BF16)) for i in range(2)])
        atrr = RR([("hat%d" % i, P.sb("hat%d" % i, [64, 512], BF16)) for i in range(2)])
        usrr = RR([("hus%d" % i, P.sb("hus%d" % i, [64, 8, 32], F32)) for i in range(2)])
        oorr = RR([("hoo%d" % i, P.sb("hoo%d" % i, [64, 8, 32], F32)) for i in range(2)])
        bcrr = RR([hb[0]])
        scrr = RR([(hb[1][0], hb[1][1][:].rearrange("p (a b) -> p a b", a=2))])
        oprr = RR([(hb[0][0], hb[0][1][:].rearrange("p (a b) -> p a b", a=2))])
        exps = RR([(hb[2][0], hb[2][1][0:64, 0:32])])
        P.capture = H_ops
        for b in range(NBAT):
            t0 = b * 512
            lk, lf = lfrr.next()
            qk, hq = hqrr.next()
            sk, sg = sgrr.next()
            vk, va = varr.next()
            P.dma(lk, lambda e, lf=lf, t0=t0: e.dma_start(out=lf[:], in_=LF_d[t0:t0 + 512, :].rearrange("(c s) k -> s c k", s=64)), writes=[lk])
            P.dma(qk, lambda e, hq=hq, t0=t0: e.dma_start(out=hq[:], in_=HQ_d[:, t0:t0 + 512]), writes=[qk])
            P.dma(sk, lambda e, sg=sg, t0=t0: e.dma_start(out=sg[:], in_=SG_d[:, t0:t0 + 512]), writes=[sk])
            P.dma(vk, lambda e, va=va, t0=t0: e.dma_start(out=va[:], in_=VA_d[t0:t0 + 512, :].rearrange("(c s) v -> s c v", s=64)), writes=[vk])
            bk, bc = bcrr.next()
            xk, xp = exps.next()
            P.pe(lambda e, bc=bc, lf=lf: [e.matmul(bc[0:64, c * 64:(c + 1) * 64], lf[:, c, :], tr[:, 0:64], start=True, stop=True) for c in range(8)],
                 reads=[lk, "tr"], writes=[bk])
            P.pe(lambda e, xp=xp, lf=lf: [e.matmul(xp[:, c * 4:(c + 1) * 4], lf[:, c, :], tr[:, 64:68], start=True, stop=True) for c in range(8)],
                 reads=[lk, "tr"], writes=[xk])
            e1k, e1 = e1rr.next()
            e2k, e2 = e2rr.next()
            exk, ex = exrr.next()
            P.act(lambda e, e1=e1, bc=bc: e.activation(out=e1[:], in_=bc[0:64, :], func=AF.Exp), reads=[bk], writes=[e1k])
            P.act(lambda e, e2=e2, bc=bc: e.activation(out=e2[:], in_=bc[0:64, :], func=AF.Exp, scale=-1.0), reads=[bk], writes=[e2k])
            P.act(lambda e, ex=ex, xp=xp: e.activation(out=ex[:].rearrange("p c f -> p (c f)"), in_=xp, func=AF.Exp), reads=[xk], writes=[exk])
            qdk, qd = qdrr.next()
            kdk, kd = kdrr.next()
            P.dve(lambda e, qd=qd, hq=hq, e1=e1: e.tensor_tensor(out=qd[:], in0=hq[:], in1=e1[:], op=ALU.mult), reads=[qk, e1k], writes=[qdk])
            P.dve(lambda e, kd=kd, sg=sg, e2=e2: e.scalar_tensor_tensor(out=kd[:], in0=sg[:], scalar=oml[:, 0:1], in1=e2[:], op0=ALU.mult, op1=ALU.mult),
                   reads=[sk, e2k, "oml"], writes=[kdk])
            P.pe(lambda e, kd=kd: [e.transpose(ptp[0:64, c * 64:(c + 1) * 64], kd[:, c * 64:(c + 1) * 64], ident[0:64, 0:64]) for c in range(8)],
                 reads=[kdk, "ident"], writes=["ptp"])
            ktk, kt = ktrr.next()
            P.act(lambda e, kt=kt: e.copy(out=kt[:], in_=ptp[0:64, 0:512]), reads=["ptp"], writes=[ktk])
            sck, sc = scrr.next()
            scv = sc[0:64, :, :].rearrange("p a b -> p (a b)")
            P.pe(lambda e, scv=scv, kd=kd, qd=qd: [e.matmul(scv[:, c * 64:(c + 1) * 64], kd[:, c * 64:(c + 1) * 64], qd[:, c * 64:(c + 1) * 64], start=True, stop=True) for c in range(8)],
                 reads=[kdk, qdk], writes=[sck])
            atk, at = atrr.next()
            P.dve(lambda e, at=at, scv=scv: e.tensor_tensor(out=at[:], in0=scv, in1=mk8f[:].rearrange("p c t -> p (c t)"), op=ALU.mult),
                  reads=[sck, "mk8f"], writes=[atk])
            uk_ = hb[2][0]
            upv = hb[2][1][0:64, 32:288]
            mk_, mi = oprr.next()
            opv = mi[0:64, 0, :]
            P.pe(lambda e, upv=upv, kt=kt, va=va: [e.matmul(upv[:, c * 32:(c + 1) * 32], kt[:, c * 64:(c + 1) * 64], va[:, c, :], start=True, stop=True) for c in range(8)],
                 reads=[ktk, vk], writes=[uk_])
            usk, us = usrr.next()
            P.dve(lambda e, us=us, upv=upv, ex=ex: e.tensor_tensor(out=us[:], in0=upv.rearrange("p (c v) -> p c v", c=8), in1=ex[:, :, 2:3].to_broadcast([64, 8, 32]), op=ALU.mult),
                  reads=[uk_, exk], writes=[usk])
            for c in range(8):
                smk, sm = smrr.next()
                P.dve(lambda e, sm=sm, ex=ex, c=c: e.tensor_scalar(out=sm[:], in0=state[:], scalar1=ex[:, c, 0:1], scalar2=None, op0=ALU.mult),
                      reads=["state", exk], writes=[smk])
                P.pe(lambda e, opv=opv, at=at, va=va, qd=qd, sm=sm, c=c: [
                    e.matmul(opv[:, c * 32:(c + 1) * 32], at[:, c * 64:(c + 1) * 64], va[:, c, :], start=True, stop=False),
                    e.matmul(opv[:, c * 32:(c + 1) * 32], qd[:, c * 64:(c + 1) * 64], sm[:], start=False, stop=True)],
                    reads=[atk, vk, qdk, smk], writes=[mk_])
                P.dve(lambda e, ex=ex, us=us, c=c: e.scalar_tensor_tensor(out=state[:], in0=state[:], scalar=ex[:, c, 1:2], in1=us[:, c, :], op0=ALU.mult, op1=ALU.add),
                      reads=["state", exk, usk], writes=["state"])
            ook, oo = oorr.next()
            P.act(lambda e, oo=oo, opv=opv: e.copy(out=oo[:].rearrange("p c v -> p (c v)"), in_=opv), reads=[mk_], writes=[ook])
            P.dma(ook, lambda e, oo=oo, t0=t0: e.dma_start(out=OH_d[t0:t0 + 512, :].rearrange("(c t) v -> t c v", t=64), in_=oo[:]), reads=[ook], writes=["oOH%d" % b])
            outs.append("oOH%d" % b)
        P.capture = None
    na, nh = len(A_ops), len(H_ops)
    ia = ih = 0
    while ia < na or ih < nh:
        if ia < na:
            P.op(*A_ops[ia]); ia += 1
        while ih < nh and (ia >= na or ih * na < ia * nh):
            P.op(*H_ops[ih]); ih += 1
    P.op("sp", None, reads=outs)
    return P.emit()

import math

D = 1024
ALPHA = 4 ** 0.25
EPS = 1e-5


def build_p3(T, layer, F, E, NJ):
    P = Prog()
    NT = T // 128
    G = T // 512
    moe = E > 1
    lam_init = 0.8 - 0.6 * math.exp(-0.3 * layer)
    x_d = P.dram("x", [T, D], F32, "ExternalInput")
    oa_d = P.dram("OATT", [T, 1024], F32, "ExternalInput")
    og_d = P.dram("OHG", [T, 256], F32, "ExternalInput")
    sg_d = P.dram("SG", [T, 256], F32, "ExternalInput")
    uh_d = P.dram("UH", [T + 128, 256], F32, "ExternalInput")
    ct_d = P.dram("ct", [128, 8], F32, "ExternalInput")
    wada_d = P.dram("wada", [D, 4096], F32, "ExternalInput")
    bada_d = P.dram("bada", [4096], F32, "ExternalInput")
    lq_d = P.dram("lamqk", [256], F32, "ExternalInput")
    ag_d = P.dram("attng", [128], F32, "ExternalInput")
    hgg_d = P.dram("hgg", [64], F32, "ExternalInput")
    pw_d = P.dram("poolw", [64, 4, 64], F32, "ExternalInput")
    psc_d = P.dram("pscale", [256], F32, "ExternalInput")
    wout_d = P.dram("wout", [D, D], F32, "ExternalInput")
    ln_d = P.dram("lnp", [4, D], F32, "ExternalInput")
    mt_d = P.dram("MT", [128, 4, 128], F32, "ExternalInput")
    mt0_d = P.dram("MT0", [128, 4, 128], F32, "ExternalInput")
    mtp_d = P.dram("MTP", [128, 4, 128], F32, "ExternalInput")
    w1_d = P.dram("w1", [E, D, F], F32, "ExternalInput")
    w3_d = P.dram("w3", [E, D, F], F32, "ExternalInput")
    w2_d = P.dram("w2", [E, F, D], F32, "ExternalInput")
    if moe:
        rw_d = P.dram("rw", [128, 8, 8], F32, "ExternalInput")
    x1_d = P.dram("x1s", [T, D], F32, "Internal")
    xo_d = P.dram("xo", [T, D], F32, "ExternalOutput")
    outs = []

    pbs = [("pb%d" % i, P.ps("pb%d" % i, [128, 512], F32)) for i in range(7)]
    prr = RR(pbs)
    psT = P.ps("psT", [128, 1024], BF16)
    ident, identf = make_ident(P)
    epst = P.sb("epst", [128, 1], F32)
    P.pool(lambda e: e.memset(epst[:], EPS), writes=["epst"])

    h2T = P.sb("h2T", [128, 8, T], BF16)
    modg2 = P.sb("modg2", [128, 1024], F32)
    lnb2 = P.sb("lnb2", [128, 2, D], F32)
    if moe:
        comb = P.sb("comb", [128, NT, 8], F32)

    P.open_scope()
    mod = P.sb("mod", [128, 3072], F32)
    lnb1 = P.sb("lnb1", [128, 2, D], F32)
    ls = P.sb("ls", [128, 2], F32)
    nlam = P.sb("nlam", [128, 1], F32)
    gA = P.sb("gA", [128, 128], F32)
    hgG = P.sb("hgG", [128, 64], F32)
    pscb = P.sb("pscb", [128, 256], F32)
    pw = P.sb("pw", [64, 4, 64], F32)
    mt = P.sb("mt", [128, 4, 128], F32)
    mt0 = P.sb("mt0", [128, 4, 128], F32)
    mtp = P.sb("mtp", [128, 4, 128], F32)
    scl8 = P.sb("scl8", [128, 8], F32)
    wob = P.sb("wob", [128, 8, D], BF16)
    if moe:
        rw = P.sb("rw_s", [128, 8, 8], F32)
    P.open_scope()
    ct = P.sb("ct_s", [128, 8], F32)
    cond = P.sb("cond", [128, 8], F32)
    condb = P.sb("condb", [128, 8, 128], F32)
    P.dma("ct", lambda e: e.dma_start(out=ct[:], in_=ct_d), writes=["ct"])
    P.act(lambda e: e.activation(out=cond[:], in_=ct[:], func=AF.Silu), reads=["ct"], writes=["cond"])
    P.dve(lambda e: e.tensor_copy(out=condb[:], in_=cond[:].unsqueeze(2).to_broadcast([128, 8, 128])), reads=["cond"], writes=["condb"])
    badab = P.sb("badab", [128, 4096], F32)
    P.dma("badab", lambda e: e.dma_start(out=badab[:], in_=bada_d.partition_broadcast(128)), writes=["badab"])
    wrr = RR([("wst%d" % i, P.sb("wst%d" % i, [128, 512], F32)) for i in range(3)])
    for n in range(8):
        pk, pb = prr.next()
        for k in range(8):
            wk, wt = wrr.next()
            P.dma(wk, lambda e, wt=wt, k=k, n=n: e.dma_start(out=wt[:], in_=wada_d[k * 128:(k + 1) * 128, n * 512:(n + 1) * 512]), writes=[wk])
            P.pe(lambda e, pb=pb, wt=wt, k=k: e.matmul(pb[:], condb[:, k, :], wt[:], start=(k == 0), stop=(k == 7)), reads=[wk, "condb"], writes=[pk])
        dst = mod[:, n * 512:(n + 1) * 512] if n < 6 else modg2[:, (n - 6) * 512:(n - 5) * 512]
        P.dve(lambda e, pb=pb, n=n, dst=dst: e.tensor_tensor(out=dst, in0=pb[:], in1=badab[:, n * 512:(n + 1) * 512], op=ALU.add),
              reads=[pk, "badab"], writes=["mod" if n < 6 else "modg2"])
    P.dve(lambda e: e.tensor_scalar_add(out=mod[:, 0:1024], in0=mod[:, 0:1024], scalar1=1.0), reads=["mod"], writes=["mod"])
    P.dve(lambda e: e.tensor_scalar_add(out=mod[:, 2048:3072], in0=mod[:, 2048:3072], scalar1=1.0), reads=["mod"], writes=["mod"])
    P.dve(lambda e: e.tensor_scalar_add(out=modg2[:], in0=modg2[:], scalar1=1.0), reads=["modg2"], writes=["modg2"])
    OPG1, SH2, OPSC2, OPG2 = mod[:, 0:1024], mod[:, 1024:2048], mod[:, 2048:3072], modg2[:]
    P.dma("lnb1", lambda e: e.dma_start(out=lnb1[:].rearrange("p a d -> p (a d)"), in_=ln_d[0:2, :].rearrange("a d -> (a d)").partition_broadcast(128)), writes=["lnb1"])
    P.dma("lnb2", lambda e: e.dma_start(out=lnb2[:].rearrange("p a d -> p (a d)"), in_=ln_d[2:4, :].rearrange("a d -> (a d)").partition_broadcast(128)), writes=["lnb2"])
    lqb = P.sb("lqb", [128, 256], F32)
    lpr = P.sb("lpr", [128, 256], F32)
    P.dma("lqb", lambda e: e.dma_start(out=lqb[:], in_=lq_d.partition_broadcast(128)), writes=["lqb"])
    lq4 = lqb[:].rearrange("p (a b d) -> p a b d", a=2, b=2)
    P.dve(lambda e: e.tensor_tensor(out=lpr[:, 0:128].rearrange("p (a d) -> p a d", a=2), in0=lq4[:, :, 0, :], in1=lq4[:, :, 1, :], op=ALU.mult), reads=["lqb"], writes=["lpr"])
    P.dve(lambda e: e.tensor_reduce(out=ls[:], in_=lpr[:, 0:128].rearrange("p (a d) -> p a d", a=2), axis=AX.X, op=ALU.add), reads=["lpr"], writes=["ls"])
    P.act(lambda e: e.activation(out=ls[:], in_=ls[:], func=AF.Exp), reads=["ls"], writes=["ls"])
    P.dve(lambda e: e.tensor_tensor(out=nlam[:], in0=ls[:, 1:2], in1=ls[:, 0:1], op=ALU.subtract), reads=["ls"], writes=["nlam"])
    P.dve(lambda e: e.tensor_scalar_add(out=nlam[:], in0=nlam[:], scalar1=-lam_init), reads=["nlam"], writes=["nlam"])
    P.dma("gA", lambda e: e.dma_start(out=gA[:], in_=ag_d.partition_broadcast(128)), writes=["gA"])
    P.dve(lambda e: e.tensor_scalar_mul(out=gA[:], in0=gA[:], scalar1=1.0 - lam_init), reads=["gA"], writes=["gA"])
    P.dma("hgG", lambda e: e.dma_start(out=hgG[:], in_=hgg_d.partition_broadcast(128)), writes=["hgG"])
    P.dma("pscb", lambda e: e.dma_start(out=pscb[:], in_=psc_d.partition_broadcast(128)), writes=["pscb"])
    P.dma("pw", lambda e: e.dma_start(out=pw[:], in_=pw_d), writes=["pw"])
    P.dma("mt", lambda e: e.dma_start(out=mt[:], in_=mt_d), writes=["mt"])
    P.dma("mt0", lambda e: e.dma_start(out=mt0[:], in_=mt0_d), writes=["mt0"])
    P.dma("mtp", lambda e: e.dma_start(out=mtp[:], in_=mtp_d), writes=["mtp"])
    P.pool(lambda e: [e.memset(scl8[:, 0:4], 1.0 / 128), e.memset(scl8[:, 4:8], 1.0 / 64)], writes=["scl8"])
    for j in range(8):
        P.dma("wob%d" % j, lambda e, j=j: e.dma_start(out=wob[:, j, :], in_=wout_d[j * 128:(j + 1) * 128, :]), writes=["wob%d" % j], eng="pool")
    wobk = ["wob%d" % j for j in range(8)]
    if moe:
        P.dma("rw", lambda e: e.dma_start(out=rw[:], in_=rw_d), writes=["rw"])

    P.close_scope()
    def rr(name, shape, dt, n=2):
        return RR([("%s%d" % (name, i), P.sb("%s%d" % (name, i), shape, dt)) for i in range(n)])
    xrr = rr("xt", [128, D], F32)
    oarr = rr("oat", [128, 1024], F32)
    ogrr = rr("ogt", [128, 256], F32)
    sgrr = rr("sgt", [128, 256], F32)
    ucrr = rr("uct", [128, 256], F32)
    uprr = rr("upt", [128, 256], F32)
    afrr = rr("af", [128, 512], F32)
    sqrr = rr("sq", [128, 512], F32)
    ssrr = rr("ss8", [128, 8], F32)
    rsrr = rr("rs8", [128, 8], F32)
    trr_ = rr("trr", [128, 256], F32)
    mixrr = rr("mix", [128, D], BF16)
    mxtrr = rr("mixT", [128, 8, 128], BF16)
    ptrr = rr("pT", [64, 512], F32)
    t1rr = rr("t1", [128, D], F32)
    y1rr = rr("y1", [128, D], F32)
    strr = rr("bst", [128, 2, 6], F32)
    mvrr = rr("mv", [128, 2], F32)
    rdrr = rr("rstd", [128, 1], F32)
    x1rr = rr("x1t", [128, D], F32)
    hfrr = rr("h2f", [128, D], F32)
    hbrr = rr("h2b", [128, D], BF16)
    if moe:
        hftrr = rr("h2fT", [128, 8, 128], F32)
        lgrr = rr("lg", [128, 8], F32)
        l2rr = rr("lg2", [128, 8], F32)
        m1rr = rr("m1", [128, 4], F32)
    for i in range(NT):
        r0 = i * 128
        xk, xt = xrr.next()
        oak, oat = oarr.next()
        ogk, ogt = ogrr.next()
        sgk, sgt = sgrr.next()
        uck, uct = ucrr.next()
        upk, upt = uprr.next()
        P.dma(xk, lambda e, xt=xt, r0=r0: e.dma_start(out=xt[:], in_=x_d[r0:r0 + 128, :]), writes=[xk])
        P.dma(oak, lambda e, oat=oat, r0=r0: e.dma_start(out=oat[:], in_=oa_d[r0:r0 + 128, :]), writes=[oak])
        P.dma(ogk, lambda e, ogt=ogt, r0=r0: e.dma_start(out=ogt[:], in_=og_d[r0:r0 + 128, :]), writes=[ogk])
        P.dma(sgk, lambda e, sgt=sgt, r0=r0: e.dma_start(out=sgt[:], in_=sg_d[r0:r0 + 128, :]), writes=[sgk])
        P.dma(uck, lambda e, uct=uct, r0=r0: e.dma_start(out=uct[:], in_=uh_d[r0 + 128:r0 + 256, :]), writes=[uck])
        P.dma(upk, lambda e, upt=upt, r0=r0: e.dma_start(out=upt[:], in_=uh_d[r0:r0 + 128, :]), writes=[upk])
        afk, af = afrr.next()
        sqk, sq = sqrr.next()
        ssk, ss8 = ssrr.next()
        rsk, rs8 = rsrr.next()
        trk, trt = trr_.next()
        mk, mix = mixrr.next()
        oa4 = oat[:].rearrange("p (h m d) -> p h m d", h=4, m=2)
        af3 = af[:].rearrange("p (h d) -> p h d", h=4)
        sq3 = sq[:].rearrange("p (h d) -> p h d", h=4)
        og3 = ogt[:].rearrange("p (h d) -> p h d", h=4)
        tr3 = trt[:].rearrange("p (h d) -> p h d", h=4)
        P.dve(lambda e, af3=af3, oa4=oa4: e.scalar_tensor_tensor(out=af3, in0=oa4[:, :, 1, :], scalar=nlam[:, 0:1], in1=oa4[:, :, 0, :], op0=ALU.mult, op1=ALU.add),
              reads=[oak, "nlam"], writes=[afk])
        P.pool(lambda e, sq=sq, af=af: e.tensor_tensor(out=sq[:], in0=af[:], in1=af[:], op=ALU.mult), reads=[afk], writes=[sqk])
        P.dve(lambda e, ss8=ss8, sq3=sq3: e.tensor_reduce(out=ss8[:, 0:4], in_=sq3, axis=AX.X, op=ALU.add), reads=[sqk], writes=[ssk])
        P.pool(lambda e, sq=sq, ogt=ogt: e.tensor_tensor(out=sq[:, 0:256], in0=ogt[:], in1=ogt[:], op=ALU.mult), reads=[ogk, ssk], writes=[sqk])
        P.dve(lambda e, ss8=ss8, sq=sq: e.tensor_reduce(out=ss8[:, 4:8], in_=sq[:, 0:256].rearrange("p (h d) -> p h d", h=4), axis=AX.X, op=ALU.add), reads=[sqk], writes=[ssk])
        P.pool(lambda e, ss8=ss8: e.tensor_tensor(out=ss8[:], in0=ss8[:], in1=scl8[:], op=ALU.mult), reads=[ssk, "scl8"], writes=[ssk])
        P.act(lambda e, rs8=rs8, ss8=ss8: e.activation(out=rs8[:], in_=ss8[:], func=AF.Sqrt, bias=epst[:], scale=1.0), reads=[ssk, "epst"], writes=[rsk])
        P.dve(lambda e, rs8=rs8: e.reciprocal(out=rs8[:], in_=rs8[:]), reads=[rsk], writes=[rsk])
        P.dve(lambda e, af3=af3, rs8=rs8: e.tensor_tensor(out=af3, in0=af3, in1=rs8[:, 0:4].unsqueeze(2).to_broadcast([128, 4, 128]), op=ALU.mult), reads=[afk, rsk], writes=[afk])
        P.pool(lambda e, mix=mix, af3=af3: e.tensor_tensor(out=mix[:, 0:512].rearrange("p (h d) -> p h d", h=4), in0=af3, in1=gA[:].unsqueeze(1).to_broadcast([128, 4, 128]), op=ALU.mult),
               reads=[afk, "gA"], writes=[mk])
        P.dve(lambda e, tr3=tr3, og3=og3, rs8=rs8: e.tensor_tensor(out=tr3, in0=og3, in1=rs8[:, 4:8].unsqueeze(2).to_broadcast([128, 4, 64]), op=ALU.mult), reads=[ogk, rsk], writes=[trk])
        P.pool(lambda e, trt=trt, sgt=sgt: e.tensor_tensor(out=trt[:], in0=trt[:], in1=sgt[:], op=ALU.mult), reads=[trk, sgk], writes=[trk])
        P.pool(lambda e, mix=mix, tr3=tr3: e.tensor_tensor(out=mix[:, 512:768].rearrange("p (h d) -> p h d", h=4), in0=tr3, in1=hgG[:].unsqueeze(1).to_broadcast([128, 4, 64]), op=ALU.mult),
               reads=[trk, "hgG"], writes=[mk])
        pk1, pb1 = prr.next()
        mtc = mt0 if i == 0 else mt
        mtck = "mt0" if i == 0 else "mt"
        P.pe(lambda e, pb1=pb1, uct=uct, upt=upt, mtc=mtc: sum([[
            e.matmul(pb1[0:64, g * 128:(g + 1) * 128], uct[:, g * 64:(g + 1) * 64], mtc[:, g, :], start=True, stop=False),
            e.matmul(pb1[0:64, g * 128:(g + 1) * 128], upt[:, g * 64:(g + 1) * 64], mtp[:, g, :], start=False, stop=True)] for g in range(4)], []),
            reads=[uck, upk, mtck, "mtp"], writes=[pk1])
        ptk, pT = ptrr.next()
        P.act(lambda e, pT=pT, pb1=pb1: e.copy(out=pT[:], in_=pb1[0:64, :]), reads=[pk1], writes=[ptk])
        pk2, pb2 = prr.next()
        P.pe(lambda e, pb2=pb2, pT=pT: [e.matmul(pb2[:, g * 64:(g + 1) * 64], pT[:, g * 128:(g + 1) * 128], pw[:, g, :], start=True, stop=True) for g in range(4)],
             reads=[ptk, "pw"], writes=[pk2])
        P.dve(lambda e, mix=mix, pb2=pb2: e.tensor_tensor(out=mix[:, 768:1024], in0=pb2[:, 0:256], in1=pscb[:], op=ALU.mult), reads=[pk2, "pscb"], writes=[mk])
        P.pe(lambda e, mix=mix: [e.transpose(psT[:, k * 128:(k + 1) * 128], mix[:, k * 128:(k + 1) * 128], ident[:]) for k in range(8)], reads=[mk, "ident"], writes=["psT"])
        mtk, mixT = mxtrr.next()
        P.act(lambda e, mixT=mixT: e.copy(out=mixT[:], in_=psT[:].rearrange("p (k t) -> p k t", k=8)), reads=["psT"], writes=[mtk])
        t1k, t1 = t1rr.next()
        for hh in range(2):
            pkm, pbm = prr.next()
            P.pe(lambda e, pbm=pbm, mixT=mixT, hh=hh: [e.matmul(pbm[:], mixT[:, k, :], wob[:, k, hh * 512:(hh + 1) * 512], start=(k == 0), stop=(k == 7)) for k in range(8)],
                 reads=[mtk] + wobk, writes=[pkm])
            P.dve(lambda e, t1=t1, pbm=pbm, hh=hh: e.tensor_tensor(out=t1[:, hh * 512:(hh + 1) * 512], in0=pbm[:], in1=OPG1[:, hh * 512:(hh + 1) * 512], op=ALU.mult),
                  reads=[pkm, "mod"], writes=[t1k])
        y1k, y1 = y1rr.next()
        P.dve(lambda e, y1=y1, xt=xt, t1=t1: e.scalar_tensor_tensor(out=y1[:], in0=xt[:], scalar=ALPHA, in1=t1[:], op0=ALU.mult, op1=ALU.add), reads=[xk, t1k], writes=[y1k])
        stk, bst = strr.next()
        mvk, mv = mvrr.next()
        rdk, rstd = rdrr.next()
        P.dve(lambda e, bst=bst, y1=y1: [e.bn_stats(out=bst[:, 0, :], in_=y1[:, 0:512]), e.bn_stats(out=bst[:, 1, :], in_=y1[:, 512:1024])], reads=[y1k], writes=[stk])
        P.dve(lambda e, mv=mv, bst=bst: e.bn_aggr(out=mv[:], in_=bst[:]), reads=[stk], writes=[mvk])
        P.act(lambda e, rstd=rstd, mv=mv: e.activation(out=rstd[:], in_=mv[:, 1:2], func=AF.Sqrt, bias=epst[:], scale=1.0), reads=[mvk, "epst"], writes=[rdk])
        P.dve(lambda e, rstd=rstd: e.reciprocal(out=rstd[:], in_=rstd[:]), reads=[rdk], writes=[rdk])
        x1k, x1t = x1rr.next()
        P.dve(lambda e, x1t=x1t, y1=y1, mv=mv, rstd=rstd: e.tensor_scalar(out=x1t[:], in0=y1[:], scalar1=mv[:, 0:1], scalar2=rstd[:, 0:1], op0=ALU.subtract, op1=ALU.mult),
              reads=[y1k, mvk, rdk], writes=[x1k])
        P.pool(lambda e, x1t=x1t: e.tensor_tensor(out=x1t[:], in0=x1t[:], in1=lnb1[:, 0, :], op=ALU.mult), reads=[x1k, "lnb1"], writes=[x1k])
        P.pool(lambda e, x1t=x1t: e.tensor_tensor(out=x1t[:], in0=x1t[:], in1=lnb1[:, 1, :], op=ALU.add), reads=[x1k, "lnb1"], writes=[x1k])
        P.dma(x1k, lambda e, x1t=x1t, r0=r0: e.dma_start(out=x1_d[r0:r0 + 128, :], in_=x1t[:]), reads=[x1k], writes=["x1d%d" % i])
        hfk, h2f = hfrr.next()
        hbk, h2b = hbrr.next()
        P.pool(lambda e, h2f=h2f, x1t=x1t: e.tensor_tensor(out=h2f[:], in0=x1t[:], in1=OPSC2, op=ALU.mult), reads=[x1k, "mod"], writes=[hfk])
        P.pool(lambda e, h2f=h2f: e.tensor_tensor(out=h2f[:], in0=h2f[:], in1=SH2, op=ALU.add), reads=[hfk, "mod"], writes=[hfk])
        P.act(lambda e, h2b=h2b, h2f=h2f: e.copy(out=h2b[:], in_=h2f[:]), reads=[hfk], writes=[hbk])
        P.pe(lambda e, h2b=h2b: [e.transpose(psT[:, k * 128:(k + 1) * 128], h2b[:, k * 128:(k + 1) * 128], ident[:]) for k in range(8)], reads=[hbk, "ident"], writes=["psT"])
        P.act(lambda e, r0=r0: e.copy(out=h2T[:, :, r0:r0 + 128], in_=psT[:].rearrange("p (k t) -> p k t", k=8)), reads=["psT"], writes=["h2T%d" % (i // 4)])
        if moe:
            hftk, h2fT = hftrr.next()
            for hh in range(2):
                pkt, pbt = prr.next()
                P.pe(lambda e, pbt=pbt, h2f=h2f, hh=hh: [e.transpose(pbt[:, kk * 128:(kk + 1) * 128], h2f[:, (hh * 4 + kk) * 128:(hh * 4 + kk + 1) * 128], identf[:]) for kk in range(4)],
                     reads=[hfk, "identf"], writes=[pkt])
                P.act(lambda e, h2fT=h2fT, pbt=pbt, hh=hh: e.copy(out=h2fT[:, hh * 4:(hh + 1) * 4, :], in_=pbt[:].rearrange("p (k t) -> p k t", k=4)), reads=[pkt], writes=[hftk])
            pkl, pbl = prr.next()
            P.pe(lambda e, pbl=pbl, h2fT=h2fT: [e.matmul(pbl[:, 0:8], h2fT[:, k, :], rw[:, k, :], start=(k == 0), stop=(k == 7)) for k in range(8)], reads=[hftk, "rw"], writes=[pkl])
            lgk, lg = lgrr.next()
            l2k, lg2 = l2rr.next()
            m1k, m1 = m1rr.next()
            P.dve(lambda e, lg=lg, pbl=pbl: e.tensor_copy(out=lg[:], in_=pbl[:, 0:8]), reads=[pkl], writes=[lgk])
            P.dve(lambda e, m1=m1, lg=lg: e.tensor_reduce(out=m1[:, 0:1], in_=lg[:], axis=AX.X, op=ALU.max), reads=[lgk], writes=[m1k])
            P.dve(lambda e, lg2=lg2, lg=lg, m1=m1: e.tensor_scalar(out=lg2[:], in0=lg[:], scalar1=m1[:, 0:1], scalar2=-1e30, op0=ALU.is_equal, op1=ALU.mult), reads=[lgk, m1k], writes=[l2k])
            P.dve(lambda e, lg2=lg2, lg=lg: e.tensor_tensor(out=lg2[:], in0=lg2[:], in1=lg[:], op=ALU.add), reads=[lgk, l2k], writes=[l2k])
            P.dve(lambda e, m1=m1, lg2=lg2: e.tensor_reduce(out=m1[:, 1:2], in_=lg2[:], axis=AX.X, op=ALU.max), reads=[l2k, m1k], writes=[m1k])
            P.dve(lambda e, lg2=lg2, lg=lg, m1=m1: e.tensor_scalar(out=lg2[:], in0=lg[:], scalar1=m1[:, 1:2], scalar2=None, op0=ALU.is_ge), reads=[lgk, m1k, l2k], writes=[l2k])
            P.dve(lambda e, m1=m1: e.tensor_scalar_mul(out=m1[:, 2:3], in0=m1[:, 0:1], scalar1=-1.0), reads=[m1k], writes=[m1k])
            P.act(lambda e, lg=lg, m1=m1: e.activation(out=lg[:], in_=lg[:], func=AF.Exp, bias=m1[:, 2:3], scale=1.0), reads=[lgk, m1k], writes=[lgk])
            P.dve(lambda e, lg=lg, lg2=lg2: e.tensor_tensor(out=lg[:], in0=lg[:], in1=lg2[:], op=ALU.mult), reads=[lgk, l2k], writes=[lgk])
            P.dve(lambda e, m1=m1, lg=lg: e.tensor_reduce(out=m1[:, 3:4], in_=lg[:], axis=AX.X, op=ALU.add), reads=[lgk, m1k], writes=[m1k])
            P.dve(lambda e, m1=m1: e.reciprocal(out=m1[:, 3:4], in_=m1[:, 3:4]), reads=[m1k], writes=[m1k])
            P.dve(lambda e, lg=lg, m1=m1, i=i: e.tensor_scalar(out=comb[:, i, :], in0=lg[:], scalar1=m1[:, 3:4], scalar2=None, op0=ALU.mult), reads=[lgk, m1k], writes=["comb"])
    P.close_scope()

    facc = P.sb("facc", [128, NT, D], F32)
    for i in range(NT):
        P.pool(lambda e, i=i: e.memset(facc[:, i, :], 0.0), writes=["facc%d" % i])
    HB = 128 * NJ
    NBLK = F // HB
    w1rr = rr("w1b", [128, 8, HB], BF16)
    w3rr = rr("w3b", [128, 8, HB], BF16)
    w2rr = rr("w2b", [128, NJ, D], BF16)
    srr = rr("sil", [128, 512], F32, 3)
    atrr = rr("actT", [128, NJ, 512], BF16)
    uprr_ = RR(pbs[0:4])
    dnrr = RR(pbs[4:7])
    pend = None

    def emit_down(item):
        ex, atk, actT, w2k, w2b, g = item
        for tt in range(4):
            ti = g * 4 + tt
            for dh in range(2):
                pkd, pbd = dnrr.next()
                P.pe(lambda e, pbd=pbd, tt=tt, dh=dh: [e.matmul(pbd[:], actT[:, j, tt * 128:(tt + 1) * 128], w2b[:, j, dh * 512:(dh + 1) * 512], start=(j == 0), stop=(j == NJ - 1)) for j in range(NJ)],
                     reads=[atk, w2k], writes=[pkd])
                fa = facc[:, ti, dh * 512:(dh + 1) * 512]
                if moe:
                    P.dve(lambda e, pbd=pbd, fa=fa, ti=ti: e.scalar_tensor_tensor(out=fa, in0=pbd[:], scalar=comb[:, ti, ex:ex + 1], in1=fa, op0=ALU.mult, op1=ALU.add),
                          reads=[pkd, "comb", "facc%d" % ti], writes=["facc%d" % ti])
                else:
                    P.dve(lambda e, pbd=pbd, fa=fa: e.tensor_tensor(out=fa, in0=pbd[:], in1=fa, op=ALU.add), reads=[pkd, "facc%d" % ti], writes=["facc%d" % ti])
    for ex in range(E):
        for hb in range(NBLK):
            c0 = hb * HB
            w1k, w1b = w1rr.next()
            w3k, w3b = w3rr.next()
            w2k, w2b = w2rr.next()
            P.dma(w1k, lambda e, w1b=w1b, ex=ex, c0=c0: e.dma_start(out=w1b[:], in_=w1_d[ex, :, c0:c0 + HB].rearrange("(k p) n -> p k n", p=128)), writes=[w1k], eng="pool")
            P.dma(w3k, lambda e, w3b=w3b, ex=ex, c0=c0: e.dma_start(out=w3b[:], in_=w3_d[ex, :, c0:c0 + HB].rearrange("(k p) n -> p k n", p=128)), writes=[w3k], eng="pool")
            P.dma(w2k, lambda e, w2b=w2b, ex=ex, c0=c0: e.dma_start(out=w2b[:], in_=w2_d[ex, c0:c0 + HB, :].rearrange("(j p) d -> p j d", p=128)), writes=[w2k], eng="pool")
            for g in range(G):
                atk, actT = atrr.next()
                for j in range(NJ):
                    pk1, pb1 = uprr_.next()
                    pk3, pb3 = uprr_.next()
                    P.pe(lambda e, pb1=pb1, w1b=w1b, j=j, g=g: [e.matmul(pb1[:], w1b[:, k, j * 128:(j + 1) * 128], h2T[:, k, g * 512:(g + 1) * 512], start=(k == 0), stop=(k == 7)) for k in range(8)],
                         reads=[w1k, "h2T%d" % g], writes=[pk1])
                    P.pe(lambda e, pb3=pb3, w3b=w3b, j=j, g=g: [e.matmul(pb3[:], w3b[:, k, j * 128:(j + 1) * 128], h2T[:, k, g * 512:(g + 1) * 512], start=(k == 0), stop=(k == 7)) for k in range(8)],
                         reads=[w3k, "h2T%d" % g], writes=[pk3])
                    sk, sil = srr.next()
                    P.act(lambda e, sil=sil, pb1=pb1: e.activation(out=sil[:], in_=pb1[:], func=AF.Silu), reads=[pk1], writes=[sk])
                    P.dve(lambda e, actT=actT, sil=sil, pb3=pb3, j=j: e.tensor_tensor(out=actT[:, j, :], in0=pb3[:], in1=sil[:], op=ALU.mult), reads=[pk3, sk], writes=[atk])
                if pend is not None:
                    emit_down(pend)
                pend = (ex, atk, actT, w2k, w2b, g)
    emit_down(pend)

    x1r = rr("x1r", [128, D], F32)
    y2r = rr("y2", [128, D], F32)
    st2 = rr("bst2", [128, 2, 6], F32)
    mv2 = rr("mv2", [128, 2], F32)
    rd2 = rr("rstd2", [128, 1], F32)
    for i in range(NT):
        r0 = i * 128
        xk, x1t = x1r.next()
        P.dma(xk, lambda e, x1t=x1t, r0=r0: e.dma_start(out=x1t[:], in_=x1_d[r0:r0 + 128, :]), reads=["x1d%d" % i], writes=[xk])
        yk, y2 = y2r.next()
        P.pool(lambda e, y2=y2, i=i: e.tensor_tensor(out=y2[:], in0=facc[:, i, :], in1=OPG2, op=ALU.mult), reads=["facc%d" % i, "modg2"], writes=[yk])
        P.dve(lambda e, y2=y2, x1t=x1t: e.scalar_tensor_tensor(out=y2[:], in0=x1t[:], scalar=ALPHA, in1=y2[:], op0=ALU.mult, op1=ALU.add), reads=[xk, yk], writes=[yk])
        stk, bst = st2.next()
        mvk, mv = mv2.next()
        rdk, rstd = rd2.next()
        P.dve(lambda e, bst=bst, y2=y2: [e.bn_stats(out=bst[:, 0, :], in_=y2[:, 0:512]), e.bn_stats(out=bst[:, 1, :], in_=y2[:, 512:1024])], reads=[yk], writes=[stk])
        P.dve(lambda e, mv=mv, bst=bst: e.bn_aggr(out=mv[:], in_=bst[:]), reads=[stk], writes=[mvk])
        P.act(lambda e, rstd=rstd, mv=mv: e.activation(out=rstd[:], in_=mv[:, 1:2], func=AF.Sqrt, bias=epst[:], scale=1.0), reads=[mvk, "epst"], writes=[rdk])
        P.dve(lambda e, rstd=rstd: e.reciprocal(out=rstd[:], in_=rstd[:]), reads=[rdk], writes=[rdk])
        P.dve(lambda e, y2=y2, mv=mv, rstd=rstd: e.tensor_scalar(out=y2[:], in0=y2[:], scalar1=mv[:, 0:1], scalar2=rstd[:, 0:1], op0=ALU.subtract, op1=ALU.mult), reads=[yk, mvk, rdk], writes=[yk])
        P.pool(lambda e, y2=y2: e.tensor_tensor(out=y2[:], in0=y2[:], in1=lnb2[:, 0, :], op=ALU.mult), reads=[yk, "lnb2"], writes=[yk])
        P.pool(lambda e, y2=y2: e.tensor_tensor(out=y2[:], in0=y2[:], in1=lnb2[:, 1, :], op=ALU.add), reads=[yk, "lnb2"], writes=[yk])
        P.dma(yk, lambda e, y2=y2, r0=r0: e.dma_start(out=xo_d[r0:r0 + 128, :], in_=y2[:]), reads=[yk], writes=["xo%d" % i])
        outs.append("xo%d" % i)
    P.op("sp", None, reads=outs)
    return P.emit()


S_FULL = 16384
NCORE = 8
TPC = S_FULL // NCORE
_PROGS = {}
_DBG = None


def _prog(key, fn):
    if key not in _PROGS:
        _PROGS[key] = fn()
    return _PROGS[key]


def _run(nc, in_maps):
    res = run_bass_kernel_spmd(nc, in_maps, core_ids=list(range(NCORE)))
    return res.results


def kernel(x, c, w_ada, b_ada, w_in, lam_qk, attn_norm_g, hg_lb_logits, hg_norm_g, pool_w, pool_scale,
           w_out, ln1_g, ln1_b, ln2_g, ln2_b, ffn_w1, ffn_w3, ffn_w2, router_w, exp_w1, exp_w3, exp_w2):
    f32 = np.float32
    A = lambda a: np.ascontiguousarray(np.asarray(a))
    x = A(x).astype(f32, copy=False)
    xs = x[0]
    ct = A(np.asarray(c, f32).reshape(8, 128).T)
    lbl = A(np.asarray(hg_lb_logits, f32))
    tric = hgrn_consts()
    mt, mtp = pool_mats(False)
    mt_first, _ = pool_mats(True)
    S_ = xs.shape[0]
    T = S_ // NCORE
    for l in range(2):
        wa = np.asarray(w_ada[l], f32)
        ba = np.asarray(b_ada[l], f32)
        nc1 = _prog(("p1", l, T), lambda: build_p1(T, l))
        winp = win_perm(np.asarray(w_in[l], f32))
        wada1 = A(wa[:, 0:2048]); bada1 = A(ba[0:2048])
        maps = []
        for ci in range(NCORE):
            cosT, sinT = rope_tables(np.arange(ci * T, (ci + 1) * T))
            maps.append({"x": A(xs[ci * T:(ci + 1) * T]), "ct": ct, "wada": wada1, "bada": bada1, "win": winp,
                         "cosT": cosT, "sinT": sinT, "lbl": lbl})
        r1 = _run(nc1, maps)
        QT = np.concatenate([r["QT"] for r in r1], 1)
        KT = np.concatenate([r["KT"] for r in r1], 1)
        V = np.concatenate([r["V"] for r in r1], 0)
        HQT = np.concatenate([r["HQT"] for r in r1], 1)
        SGT = np.concatenate([r["SGT"] for r in r1], 1)
        LOGF = np.concatenate([r["LOGF"] for r in r1], 0)
        VAL = np.concatenate([r["VAL"] for r in r1], 0)
        SG = np.concatenate([r["SG"] for r in r1], 0)
        U = np.concatenate([r["U"] for r in r1], 0)
        nc2 = _prog(("p2", l, S_), lambda: build_p2(S_, True, True, l))
        maps = []
        for n in range(NCORE):
            h, z = n // 2, n % 2
            maps.append({"QTn": A(QT[n * 64:(n + 1) * 64]), "KTn": A(KT[n * 64:(n + 1) * 64]), "Vh": A(V[:, h * 128:(h + 1) * 128]),
                         "HQTh": A(HQT[h * 64:(h + 1) * 64]), "SGTh": A(SGT[h * 64:(h + 1) * 64]), "LOGFh": A(LOGF[:, h * 64:(h + 1) * 64]),
                         "VALhz": A(VAL[:, h * 64 + z * 32:h * 64 + z * 32 + 32]), "lblh": A(lbl[:, h * 64:(h + 1) * 64].T), "TRIC": tric})
        r2 = _run(nc2, maps)
        OATT = np.concatenate([np.ascontiguousarray(np.asarray(r["OATT"]).T) for r in r2], 1)
        OHG = np.concatenate([r["OHG"] for r in r2], 1)
        if _DBG is not None:
            _DBG[l] = dict(QT=QT, KT=KT, V=V, HQT=HQT, SGT=SGT, LOGF=LOGF, VAL=VAL, SG=SG, U=U, OATT=OATT, OHG=OHG)
        moe = (l % 2 == 1)
        if moe:
            E, F, NJ = 8, 3584, 4
            w1 = np.asarray(exp_w1[l // 2], f32); w3 = np.asarray(exp_w3[l // 2], f32); w2 = np.asarray(exp_w2[l // 2], f32)
        else:
            E, F, NJ = 1, 2816, 2
            w1 = np.asarray(ffn_w1[l // 2], f32)[None]; w3 = np.asarray(ffn_w3[l // 2], f32)[None]; w2 = np.asarray(ffn_w2[l // 2], f32)[None]
        nc3 = _prog(("p3", l, T), lambda: build_p3(T, l, F, E, NJ))
        UH = np.concatenate([np.zeros((128, 256), f32), U], 0)
        base = {"ct": ct, "wada": A(wa[:, 2048:]), "bada": A(ba[2048:]), "lamqk": A(np.asarray(lam_qk[l], f32).reshape(-1)),
                "attng": A(np.asarray(attn_norm_g[l], f32)), "hgg": A(np.asarray(hg_norm_g[l], f32)),
                "poolw": A(np.asarray(pool_w[l], f32).transpose(1, 0, 2)), "pscale": A(np.asarray(pool_scale[l], f32)),
                "wout": A(np.asarray(w_out[l], f32)),
                "lnp": A(np.stack([np.asarray(ln1_g[l], f32), np.asarray(ln1_b[l], f32), np.asarray(ln2_g[l], f32), np.asarray(ln2_b[l], f32)])),
                "MT": mt, "MTP": mtp, "w1": A(w1), "w3": A(w3), "w2": A(w2)}
        if moe:
            base["rw"] = A(np.asarray(router_w[l // 2], f32).reshape(8, 128, 8).transpose(1, 0, 2))
        maps = []
        for ci in range(NCORE):
            m = dict(base)
            sl = slice(ci * T, (ci + 1) * T)
            m.update({"x": A(xs[sl]), "OATT": A(OATT[sl]), "OHG": A(OHG[sl]), "SG": A(SG[sl]), "UH": A(UH[ci * T:(ci + 1) * T + 128]),
                      "MT0": mt_first if ci == 0 else mt})
            maps.append(m)
        r3 = _run(nc3, maps)
        xs = np.concatenate([r["xo"] for r in r3], 0)
        if _DBG is not None:
            _DBG[l]["xo"] = xs
    return xs[None].astype(f32, copy=False)
```
